# Optimizing a Trainium2 kernel written in Bass

```python
import jax, jax.numpy as jnp
from jax import lax
import numpy as np

D_MODEL = 2048
BATCH = 1
SEQ = 16384
DEPTH = 1

D_MIX = 2 * D_MODEL
D_SSD = 3 * D_MIX // 4
SSD_HEAD_DIM = 64
SSD_HEADS = D_SSD // SSD_HEAD_DIM
SSD_GROUPS = 4
SSD_HEADS_PER_GROUP = SSD_HEADS // SSD_GROUPS
D_STATE = 128
CONV_K = 4
CHUNK = 128
D_CONV = D_SSD + 2 * SSD_GROUPS * D_STATE
D_POOL = D_MIX - D_SSD
POOL_WINDOWS = (2, 4, 8, 16)
POOL_GROUPS = len(POOL_WINDOWS)
POOL_GROUP = D_POOL // POOL_GROUPS
D_IN_PROJ = D_SSD + D_CONV + SSD_HEADS + D_POOL
N_EXPERTS = 32
TOP_K = 4
D_EXPERT = D_MODEL
SWIGLU_LIMIT = 7.0
SWIGLU_ALPHA = 1.702
MOE_BLOCK = 128
NORM_EPS = 1e-6
N_MOD = 6

kernel_name = 'hybrid_ssd_pool_moe_adaln'


def rms_norm(x, g):
    xf = x.astype(jnp.float32)
    y = xf * lax.rsqrt(jnp.mean(xf * xf, axis=-1, keepdims=True) + NORM_EPS)
    return (y * g.astype(jnp.float32)).astype(x.dtype)


def causal_depthwise_conv(u, w, b):
    y = lax.conv_general_dilated(u, w[:, None, :], window_strides=(1,), padding=[(CONV_K - 1, 0)],
                                 dimension_numbers=('NWC', 'WIO', 'NWC'), feature_group_count=u.shape[-1])
    return y + b


def ssd_chunked_scan(x, dt, a, bm, cm):
    b, s, _, p = x.shape
    g, r, n = SSD_GROUPS, SSD_HEADS_PER_GROUP, D_STATE
    nc = s // CHUNK
    xs = jnp.moveaxis(x.reshape(b, nc, CHUNK, g, r, p), 1, 0)
    dts = jnp.moveaxis(dt.reshape(b, nc, CHUNK, g, r), 1, 0)
    das = dts * a.reshape(g, r)
    bs = jnp.moveaxis(bm.reshape(b, nc, CHUNK, g, n), 1, 0)
    cs_ = jnp.moveaxis(cm.reshape(b, nc, CHUNK, g, n), 1, 0)
    causal = jnp.tril(jnp.ones((CHUNK, CHUNK), dtype=bool))[None, :, :, None, None]

    def step(state, inp):
        xc, dtc, dac, bc, cc = inp
        cum = jnp.cumsum(dac, axis=1)
        seg = cum[:, :, None] - cum[:, None, :]
        decay = jnp.exp(jnp.where(causal, seg, -jnp.inf))
        cb = jnp.einsum('blgn,bsgn->blsg', cc, bc)
        m = cb[..., None] * decay * dtc[:, None]
        y_diag = jnp.einsum('blsgr,bsgrp->blgrp', m, xc)
        y_off = jnp.einsum('blgn,bgrpn->blgrp', cc, state) * jnp.exp(cum)[..., None]
        w_end = jnp.exp(cum[:, -1:] - cum) * dtc
        new_state = state * jnp.exp(cum[:, -1])[..., None, None] + jnp.einsum('bsgn,bsgr,bsgrp->bgrpn', bc, w_end, xc)
        return new_state, y_diag + y_off

    state0 = jnp.zeros((b, g, r, p, n), jnp.float32)
    _, ys = lax.scan(step, state0, (xs, dts, das, bs, cs_))
    return jnp.moveaxis(ys, 0, 1).reshape(b, s, SSD_HEADS, p)


def causal_multiscale_pool(u):
    b, s, _ = u.shape
    uf = u.astype(jnp.float32).reshape(b, s, POOL_GROUPS, POOL_GROUP)
    cs = jnp.concatenate([jnp.zeros((b, 1, POOL_GROUPS, POOL_GROUP), jnp.float32), jnp.cumsum(uf, axis=1)], axis=1)
    hi = jnp.arange(1, s + 1)
    outs = []
    for gi, w in enumerate(POOL_WINDOWS):
        csg = cs[:, :, gi]
        lo = jnp.maximum(hi - w, 0)
        win_sum = csg[:, 1:] - csg[:, lo]
        count = jnp.minimum(hi, w).astype(jnp.float32)[None, :, None]
        outs.append(win_sum / count - uf[:, :, gi])
    return jnp.stack(outs, axis=2)


def parallel_mixer(h, w_in_proj, conv_w, conv_b, dt_bias, a_log, d_skip, ssd_norm_g, w_pool, b_pool, pool_scale, w_out_proj):
    b, s, _ = h.shape
    proj = h @ w_in_proj
    z, xbc, dt_raw, u = jnp.split(proj, [D_SSD, D_SSD + D_CONV, D_SSD + D_CONV + SSD_HEADS], axis=-1)
    xbc = jax.nn.silu(causal_depthwise_conv(xbc, conv_w, conv_b))
    xs, bm, cm = jnp.split(xbc, [D_SSD, D_SSD + SSD_GROUPS * D_STATE], axis=-1)
    xs = xs.astype(jnp.float32).reshape(b, s, SSD_HEADS, SSD_HEAD_DIM)
    dt = jax.nn.softplus(dt_raw.astype(jnp.float32) + dt_bias.astype(jnp.float32))
    a = -jnp.exp(a_log.astype(jnp.float32))
    y = ssd_chunked_scan(xs, dt, a,
                         bm.astype(jnp.float32).reshape(b, s, SSD_GROUPS, D_STATE),
                         cm.astype(jnp.float32).reshape(b, s, SSD_GROUPS, D_STATE))
    y = y + d_skip.astype(jnp.float32)[:, None] * xs
    y = y.reshape(b, s, D_SSD) * jax.nn.silu(z.astype(jnp.float32))
    yg = y.reshape(b, s, SSD_GROUPS, D_SSD // SSD_GROUPS)
    yg = yg * lax.rsqrt(jnp.mean(yg * yg, axis=-1, keepdims=True) + NORM_EPS)
    y_ssd = (yg.reshape(b, s, D_SSD) * ssd_norm_g.astype(jnp.float32)).astype(h.dtype)
    pooled = causal_multiscale_pool(u).astype(h.dtype)
    y_pool = jnp.einsum('bsgc,gcd->bsgd', pooled, w_pool).reshape(b, s, D_POOL)
    y_pool = (y_pool + b_pool) * pool_scale
    return jnp.concatenate([y_ssd, y_pool], axis=-1) @ w_out_proj


def moe_block(h, w_router, b_router, w_exp_in, b_exp_in, w_exp_out, b_exp_out):
    b, s, d = h.shape
    t = b * s
    hf = h.reshape(t, d)
    logits = (hf @ w_router).astype(jnp.float32) + b_router.astype(jnp.float32)
    top_val, top_idx = lax.top_k(logits, TOP_K)
    gates = jax.nn.softmax(top_val, axis=-1)
    n_assign = t * TOP_K
    e_flat = top_idx.reshape(-1)
    g_flat = gates.reshape(-1)
    tok_flat = jnp.arange(n_assign, dtype=jnp.int32) // TOP_K
    order = jnp.argsort(e_flat)
    e_sorted = e_flat[order]
    counts = jnp.bincount(e_flat, length=N_EXPERTS)
    padded = (counts + MOE_BLOCK - 1) // MOE_BLOCK * MOE_BLOCK
    ustart = jnp.cumsum(counts) - counts
    pend = jnp.cumsum(padded)
    pstart = pend - padded
    dest = pstart[e_sorted] + jnp.arange(n_assign) - ustart[e_sorted]
    capacity = n_assign + N_EXPERTS * MOE_BLOCK
    n_blocks = capacity // MOE_BLOCK
    row_tok = jnp.zeros((capacity,), jnp.int32).at[dest].set(tok_flat[order])
    row_gate = jnp.zeros((capacity,), jnp.float32).at[dest].set(g_flat[order])
    block_start = jnp.arange(n_blocks) * MOE_BLOCK
    block_expert = jnp.minimum(jnp.searchsorted(pend, block_start, side='right'), N_EXPERTS - 1)

    def expert_rows(args):
        e, tok, gate = args
        xb = hf[tok]
        hb = xb @ w_exp_in[e] + b_exp_in[e]
        glu = jnp.minimum(hb[:, ::2], SWIGLU_LIMIT)
        lin = jnp.clip(hb[:, 1::2], -SWIGLU_LIMIT, SWIGLU_LIMIT)
        act = glu * jax.nn.sigmoid(SWIGLU_ALPHA * glu) * (lin + 1)
        return (act @ w_exp_out[e] + b_exp_out[e]) * gate.astype(h.dtype)[:, None]

    out = lax.map(expert_rows, (block_expert, row_tok.reshape(n_blocks, MOE_BLOCK), row_gate.reshape(n_blocks, MOE_BLOCK)))
    y = jax.ops.segment_sum(out.reshape(capacity, d), row_tok, num_segments=t)
    return y.reshape(b, s, d)


def setup_inputs(seed: int = 0) -> dict:
    key = jax.random.key(seed)
    ks = jax.random.split(key, 24)
    f32 = jnp.float32

    def nrm(k, shape, scale):
        return jax.random.normal(k, shape, f32) * scale

    dt_init = jnp.exp(jax.random.uniform(ks[6], (DEPTH, SSD_HEADS), f32, np.log(1e-3), np.log(1e-1)))
    dt_bias = dt_init + jnp.log(-jnp.expm1(-dt_init))
    a_log = jnp.log(jax.random.uniform(ks[7], (DEPTH, SSD_HEADS), f32, 1.0, 16.0))
    return {
        'x': nrm(ks[0], (BATCH, SEQ, D_MODEL), 1.0),
        'c': nrm(ks[1], (BATCH, D_MODEL), 1.0),
        'w_ada': nrm(ks[2], (DEPTH, D_MODEL, N_MOD * D_MODEL), 0.5 * D_MODEL ** -0.5),
        'b_ada': nrm(ks[3], (DEPTH, N_MOD * D_MODEL), 0.02),
        'norm1_g': 1.0 + nrm(ks[4], (DEPTH, D_MODEL), 0.02),
        'w_in_proj': nrm(ks[5], (DEPTH, D_MODEL, D_IN_PROJ), D_MODEL ** -0.5),
        'conv_w': nrm(ks[8], (DEPTH, CONV_K, D_CONV), CONV_K ** -0.5),
        'conv_b': nrm(ks[9], (DEPTH, D_CONV), 0.02),
        'dt_bias': dt_bias,
        'a_log': a_log,
        'd_skip': 1.0 + nrm(ks[10], (DEPTH, SSD_HEADS), 0.1),
        'ssd_norm_g': 1.0 + nrm(ks[11], (DEPTH, D_SSD), 0.02),
        'w_pool': nrm(ks[12], (DEPTH, POOL_GROUPS, POOL_GROUP, POOL_GROUP), POOL_GROUP ** -0.5),
        'b_pool': nrm(ks[13], (DEPTH, D_POOL), 0.02),
        'pool_scale': 1.0 + nrm(ks[14], (DEPTH, D_POOL), 0.1),
        'w_out_proj': nrm(ks[15], (DEPTH, D_MIX, D_MODEL), D_MIX ** -0.5),
        'norm2_g': 1.0 + nrm(ks[16], (DEPTH, D_MODEL), 0.02),
        'w_router': nrm(ks[17], (DEPTH, D_MODEL, N_EXPERTS), D_MODEL ** -0.5),
        'b_router': nrm(ks[18], (DEPTH, N_EXPERTS), 0.01),
        'w_exp_in': nrm(ks[19], (DEPTH, N_EXPERTS, D_MODEL, 2 * D_EXPERT), D_MODEL ** -0.5),
        'b_exp_in': nrm(ks[20], (DEPTH, N_EXPERTS, 2 * D_EXPERT), 0.02),
        'w_exp_out': nrm(ks[21], (DEPTH, N_EXPERTS, D_EXPERT, D_MODEL), D_EXPERT ** -0.5),
        'b_exp_out': nrm(ks[22], (DEPTH, N_EXPERTS, D_MODEL), 0.02),
        'final_norm_g': 1.0 + nrm(ks[23], (D_MODEL,), 0.02),
    }


def reference(x, c, w_ada, b_ada, norm1_g, w_in_proj, conv_w, conv_b, dt_bias, a_log, d_skip, ssd_norm_g,
              w_pool, b_pool, pool_scale, w_out_proj, norm2_g, w_router, b_router, w_exp_in, b_exp_in,
              w_exp_out, b_exp_out, final_norm_g):
    cond = jax.nn.silu(c)
    for i in range(DEPTH):
        mod = (cond @ w_ada[i] + b_ada[i])[:, None, :]
        sh1, sc1, g1, sh2, sc2, g2 = jnp.split(mod, N_MOD, axis=-1)
        h = rms_norm(x, norm1_g[i]) * (1 + sc1) + sh1
        x = x + g1 * parallel_mixer(h, w_in_proj[i], conv_w[i], conv_b[i], dt_bias[i], a_log[i], d_skip[i],
                                    ssd_norm_g[i], w_pool[i], b_pool[i], pool_scale[i], w_out_proj[i])
        h = rms_norm(x, norm2_g[i]) * (1 + sc2) + sh2
        x = x + g2 * moe_block(h, w_router[i], b_router[i], w_exp_in[i], b_exp_in[i], w_exp_out[i], b_exp_out[i])
    return rms_norm(x, final_norm_g)
```

```python
import contextlib
import numpy as np
import concourse.bass as bass
import concourse.mybir as mybir
from concourse.bass_utils import run_bass_kernel_spmd

F32 = mybir.dt.float32
BF16 = mybir.dt.bfloat16
AF = mybir.ActivationFunctionType
ALU = mybir.AluOpType

D = 2048
KD = 16
D_SSD = 3072
NH = 48
HD = 64
NG = 4
HPG = 12
DST = 128
D_CONV = 4096
D_POOL = 1024
D_IN = 8240
OFF_XBC = 3072
OFF_DT = 7168
OFF_U = 7216
F_EXP = 2048
EPS = 1e-6
LIMIT = 7.0
ALPHA = 1.702
GT = 256

PHASE = 24000
SAME_ENGINE_RAW = True


class Buf:
    __slots__ = ("name", "lw", "rd", "excl")

    def __init__(self, name, excl=False):
        self.name = name
        self.excl = excl
        self.lw = None
        self.rd = []


class Op:
    __slots__ = ("eng", "fn", "is_dma", "key", "deps", "sig", "tok", "drain")

    def __init__(self, eng, fn, is_dma, key):
        self.eng = eng
        self.fn = fn
        self.is_dma = is_dma
        self.key = key
        self.deps = ()
        self.sig = False
        self.tok = None
        self.drain = False


class Prog:
    def __init__(self, nc, stack):
        self.nc = nc
        self.stack = stack
        self.ops = []
        self.h = {"pe": nc.tensor, "act": nc.scalar, "dve": nc.vector,
                  "pool": nc.gpsimd, "sp": nc.sync}
        self.nbuf = 0

    def buf(self, name=None, excl=False):
        self.nbuf += 1
        return Buf(name or f"b{self.nbuf}", excl)

    def bufs(self, n, name="b", excl=False):
        return [self.buf(f"{name}{i}", excl) for i in range(n)]

    def _add(self, op, r, w):
        i = len(self.ops)
        xr = [b for b in r if b.excl]
        if xr:
            w = list(w) + [b for b in xr if b not in w]
        raw = set()
        oth = set()
        for b in r:
            if b.lw is not None:
                raw.add(b.lw)
        for b in w:
            if b.lw is not None:
                oth.add(b.lw)
            for x in b.rd:
                oth.add(x)
        oth -= raw
        oth.discard(i)
        op.deps = tuple((d, True) for d in sorted(raw)) + tuple((d, False) for d in sorted(oth))
        self.ops.append(op)
        for b in r:
            b.rd.append(i)
        for b in w:
            b.lw = i
            b.rd = []
        return i

    def op(self, eng, fn, r=(), w=()):
        return self._add(Op(eng, fn, False, None), r, w)

    def dma(self, eng, out, in_, r=(), w=(), key=None, **kw):
        def fn(h):
            return h.dma_start(out=out, in_=in_, **kw)
        return self._add(Op(eng, fn, True, key or "dflt_" + eng), r, w)

    def barrier(self, scratch, pbuf=()):
        bs = {}
        for e in ("pe", "act", "dve", "pool", "sp"):
            bs[e] = self.buf("bar_" + e)
        sc = scratch
        zr = [sc["zbuf"]]
        self.op("act", lambda h: h.activation(out=sc["a"][:, 0:1], in_=sc["z"][:, 0:1], func=AF.Copy), r=zr, w=[bs["act"]])
        self.op("dve", lambda h: h.tensor_copy(out=sc["v"][:, 0:1], in_=sc["z"][:, 0:1]), r=zr, w=[bs["dve"]])
        self.op("pool", lambda h: h.tensor_copy(out=sc["g"][:, 0:1], in_=sc["z"][:, 0:1]), r=zr, w=[bs["pool"]])
        self.op("pe", lambda h: h.matmul(sc["p"][0:1, 0:1], lhsT=sc["zb"][:, 0:1], rhs=sc["zb"][:, 0:1], start=True, stop=True), r=zr, w=[bs["pe"]] + list(pbuf))
        i = self.op("sp", lambda h: h.nop(), w=[bs["sp"]])
        self.ops[i].drain = True
        allb = list(bs.values())
        self.op("act", lambda h: h.activation(out=sc["a"][:, 1:2], in_=sc["z"][:, 0:1], func=AF.Copy), r=allb)
        self.op("dve", lambda h: h.tensor_copy(out=sc["v"][:, 1:2], in_=sc["z"][:, 0:1]), r=allb)
        self.op("pool", lambda h: h.tensor_copy(out=sc["g"][:, 1:2], in_=sc["z"][:, 0:1]), r=allb)
        self.op("pe", lambda h: h.matmul(sc["p"][0:1, 1:2], lhsT=sc["zb"][:, 0:1], rhs=sc["zb"][:, 0:1], start=True, stop=True), r=allb, w=list(pbuf))
        self.op("sp", lambda h: h.nop(), r=allb)

    def emit(self, final_wait_eng="sp"):
        nc, ops = self.nc, self.ops
        for i, op in enumerate(ops):
            for d, israw in op.deps:
                p = ops[d]
                if p.is_dma:
                    p.sig = True
                elif p.eng != op.eng or op.is_dma:
                    p.sig = True
                elif SAME_ENGINE_RAW and p.eng != "pe":
                    p.sig = True
        for op in ops:
            if op.is_dma:
                op.sig = True
        sems = {}

        def getsem(name):
            if name not in sems:
                sems[name] = self.stack.enter_context(nc.semaphore(name))
            return sems[name]

        cnt = {}
        waited = {}
        n_wait = 0
        for i, op in enumerate(ops):
            h = self.h[op.eng]
            need = {}
            for d, israw in op.deps:
                p = ops[d]
                if p.tok is None:
                    continue
                if (not p.is_dma) and p.eng == op.eng and not op.is_dma:
                    if not (SAME_ENGINE_RAW and p.eng != "pe"):
                        continue
                sn, v = p.tok
                if need.get(sn, 0) < v:
                    need[sn] = v
            if op.drain:
                for sn in sems:
                    if sn.startswith("d_"):
                        need[sn] = max(need.get(sn, 0), cnt[sn])
            for sn, v in need.items():
                if waited.get((op.eng, sn), 0) >= v:
                    continue
                h.wait_ge(sems[sn], v)
                waited[(op.eng, sn)] = v
                n_wait += 1
            ins = op.fn(h)
            if op.sig:
                if op.is_dma:
                    sn = "d_" + op.key
                    inc = 16
                else:
                    base = "e_" + op.eng
                    ph = cnt.get(base + "_n", 0) // PHASE
                    cnt[base + "_n"] = cnt.get(base + "_n", 0) + 1
                    sn = f"{base}{ph}"
                    inc = 1
                s = getsem(sn)
                cnt[sn] = cnt.get(sn, 0) + inc
                ins.then_inc(s, inc)
                op.tok = (sn, cnt[sn])
        h = self.h[final_wait_eng]
        for sn, s in sems.items():
            if sn.startswith("d_"):
                if waited.get((final_wait_eng, sn), 0) < cnt[sn]:
                    h.wait_ge(s, cnt[sn])
        self.stats = dict(n_ops=len(ops), n_wait=n_wait, n_sems=len(sems))
        return self.stats


class Cfg:
    def __init__(self, seq, ncores, n_exp, pt=1024):
        self.SEQ = seq
        self.NC = ncores
        self.NE = n_exp
        self.T = seq // ncores
        self.TP = (ncores - 1) * self.T
        self.PT = min(pt, self.T)
        assert self.T % GT == 0 and self.T % self.PT == 0


def build_program(cfg, stop=9, lvl=9):
    T, TP, NE, PT = cfg.T, cfg.TP, cfg.NE, cfg.PT
    NTILE = T // 128
    nc = bass.Bass("TRN2", target_bir_lowering=False)

    def din(name, shape, dt=F32):
        return nc.dram_tensor(name, list(shape), dt, kind="ExternalInput").ap()

    x_own = din("x_own", [T, D])
    x_prev = din("x_prev", [max(TP, 128), D])
    pmask = din("pmask", [128, max(TP, 128) // 128])
    lastv = din("lastv", [128, 1])
    invc = din("invc", [4, GT])
    c_T = din("c_T", [128, KD])
    w_ada = din("w_ada", [D, 6 * D])
    b_ada = din("b_ada", [1, 6 * D])
    n1g_T = din("n1g_T", [128, KD])
    n2g_T = din("n2g_T", [128, KD])
    w_in = din("w_in", [D, D_IN])
    conv_wT = din("conv_wT", [128, 32, 4])
    conv_bT = din("conv_bT", [128, 32])
    dt_bias = din("dt_bias", [1, NH])
    a_log = din("a_log", [1, NH])
    d_skip = din("d_skip", [1, NH])
    ssd_g = din("ssd_g", [1, D_SSD])
    w_pool = din("w_pool", [4, 256, 256])
    b_poolT = din("b_poolT", [128, 8])
    pscaleT = din("pscaleT", [128, 8])
    w_out = din("w_out", [2 * D, D])
    w_router = din("w_router", [D, NE])
    b_router = din("b_router", [1, NE])
    w_ein = din("w_ein", [NE, D, 2 * F_EXP])
    b_einT = din("b_einT", [128, NE, 16, 2])
    w_eout = din("w_eout", [NE, F_EXP, D])
    b_eout = din("b_eout", [NE, D])
    fng = din("fng", [1, D])
    identd = din("ident", [128, 128])
    utd = din("ut_c", [128, 128])
    mld = din("ml_c", [128, 128])
    out = nc.dram_tensor("out", [T, D], F32, kind="ExternalOutput").ap()
    x1_scr = nc.dram_tensor("x1_scr", [T, D], F32).ap()
    h2T_scr = nc.dram_tensor("h2T_scr", [128, KD, T], BF16).ap()
    g2_scr = nc.dram_tensor("g2_scr", [128, D], F32).ap()

    with contextlib.ExitStack() as top:
        P = Prog(nc, top)

        def SB(st, name, shape, dt):
            return st.enter_context(nc.sbuf_tensor(name, list(shape), dt))

        def PS(st, name, shape, dt):
            return st.enter_context(nc.psum_tensor(name, list(shape), dt))

        identf = SB(top, "identf", [128, 128], F32)
        identb = SB(top, "identb", [128, 128], BF16)
        A1 = SB(top, "A1", [128, KD], F32)
        B1 = SB(top, "B1", [128, KD], F32)
        A2 = SB(top, "A2", [128, KD], F32)
        B2 = SB(top, "B2", [128, KD], F32)
        gB = SB(top, "gB", [128, D], F32)
        gates = SB(top, "gates", [128, NTILE, NE], F32)
        barsc = dict(a=SB(top, "bar_a", [128, 2], F32), v=SB(top, "bar_v", [128, 2], F32),
                     g=SB(top, "bar_g", [128, 2], F32), z=SB(top, "bar_z", [128, 2], F32),
                     zb=SB(top, "bar_zb", [128, 2], BF16))
        b_identf, b_identb, b_A1, b_B1, b_A2, b_B2, b_gB, b_barz = P.bufs(8, "pers")
        b_gates = P.bufs(NTILE, "gates")
        P.dma("sp", identf[:], identd[:, :], w=[b_identf], key="c0")
        P.op("dve", lambda h: h.tensor_copy(out=identb[:], in_=identf[:]), r=[b_identf], w=[b_identb])
        P.op("dve", lambda h: h.memset(barsc["z"][:], 0.0), w=[b_barz])
        P.op("dve", lambda h: h.memset(barsc["zb"][:], 0.0), w=[b_barz])
        barsc["zbuf"] = b_barz

        with contextlib.ExitStack() as st:
            barsc["p"] = PS(st, "bar_p0", [128, 512], F32)
            b_barp0 = P.buf("barp0", excl=True)
            cT = SB(st, "cT", [128, KD], F32)
            condT = SB(st, "condT", [128, KD], F32)
            condB = SB(st, "condB", [128, KD, 128], F32)
            wa = [SB(st, f"wa{i}", [128, KD, 256], F32) for i in range(2)]
            modB = SB(st, "modB", [128, 6 * D], F32)
            baB = SB(st, "baB", [128, 6 * D], F32)
            modT = SB(st, "modT", [128, 4, KD], F32)
            ngT = SB(st, "ngT", [128, 2, KD], F32)
            pm = [PS(st, f"pm{i}", [128, 512], F32) for i in range(2)]
            ptr = [PS(st, f"ptr{i}", [128, 512], F32) for i in range(2)]
            b_cT, b_condT, b_condB, b_baB, b_modT, b_ngT = P.bufs(6, "p0")
            b_wa = P.bufs(2, "wa")
            b_pm = P.bufs(2, "pm", excl=True)
            b_ptr = P.bufs(2, "ptr", excl=True)
            b_modB = P.bufs(48, "modB")
            P.dma("sp", cT[:], c_T[:, :], w=[b_cT], key="c1")
            P.dma("sp", ngT[:, 0, :], n1g_T[:, :], w=[b_ngT], key="c2")
            P.dma("sp", ngT[:, 1, :], n2g_T[:, :], w=[b_ngT], key="c2")
            P.dma("act", baB[:], b_ada[0:1, :].broadcast_to([128, 6 * D]), w=[b_baB], key="c3")
            P.op("act", lambda h: h.activation(out=condT[:], in_=cT[:], func=AF.Silu), r=[b_cT], w=[b_condT])
            for kc in range(KD):
                P.op("dve", lambda h, kc=kc: h.tensor_copy(out=condB[:, kc, :], in_=condT[:, kc:kc + 1].to_broadcast([128, 128])),
                     r=[b_condT], w=[b_condB])
            if stop == -3:
                P.dma("sp", out[0:128, 0:128], condB[:, 3, :], r=[b_condB], key="dbg")
                P.dma("sp", out[128:256, :], baB[:, 0:D], r=[b_baB], key="dbg1")
                return nc, P.emit()
            for n in range(48 if stop != -2 else 1):
                s = n % 2
                P.dma("sp" if n % 2 == 0 else "act", wa[s][:],
                      w_ada[:, n * 256:(n + 1) * 256].rearrange("(kc p) c -> p kc c", p=128),
                      w=[b_wa[s]], key=f"wa{s}")
                for kc in range(KD):
                    P.op("pe", lambda h, kc=kc, s=s: h.matmul(pm[s][:, 0:256], lhsT=condB[:, kc, :], rhs=wa[s][:, kc, :],
                                                              start=(kc == 0), stop=(kc == KD - 1)),
                         r=[b_condB, b_wa[s]], w=[b_pm[s]])
                P.op("dve", lambda h, n=n, s=s: h.tensor_tensor(out=modB[:, n * 256:(n + 1) * 256], in0=pm[s][:, 0:256],
                                                                 in1=baB[:, n * 256:(n + 1) * 256], op=ALU.add),
                     r=[b_pm[s], b_baB], w=[b_modB[n]])
            if stop in (-2, -1):
                P.dma("sp", out[0:128, :], modB[:, 0:D], r=b_modB[0:8], key="dbg")
                return nc, P.emit()
            for vi, v in enumerate((0, 1, 3, 4)):
                for j in range(KD):
                    col = v * D + j * 128
                    s = (vi * KD + j) % 2
                    P.op("pe", lambda h, col=col, s=s: h.matmul(ptr[s][:, 0:128], lhsT=modB[:, col:col + 128], rhs=identf[:], start=True, stop=True),
                         r=[b_modB[col // 256], b_identf], w=[b_ptr[s]])
                    P.op("dve", lambda h, vi=vi, j=j, s=s: h.tensor_copy(out=modT[:, vi, j:j + 1], in_=ptr[s][:, 0:1]),
                         r=[b_ptr[s]], w=[b_modT])
            P.op("dve", lambda h: h.scalar_tensor_tensor(out=A1[:], in0=modT[:, 1, :], scalar=1.0, in1=ngT[:, 0, :], op0=ALU.add, op1=ALU.mult),
                 r=[b_modT, b_ngT], w=[b_A1])
            P.op("dve", lambda h: h.tensor_copy(out=B1[:], in_=modT[:, 0, :]), r=[b_modT], w=[b_B1])
            P.op("dve", lambda h: h.scalar_tensor_tensor(out=A2[:], in0=modT[:, 3, :], scalar=1.0, in1=ngT[:, 1, :], op0=ALU.add, op1=ALU.mult),
                 r=[b_modT, b_ngT], w=[b_A2])
            P.op("dve", lambda h: h.tensor_copy(out=B2[:], in_=modT[:, 2, :]), r=[b_modT], w=[b_B2])
            P.op("dve", lambda h: h.tensor_copy(out=gB[:], in_=modB[:, 2 * D:3 * D]), r=b_modB[16:24], w=[b_gB])
            P.dma("sp", g2_scr[:, :], modB[:, 5 * D:6 * D], r=b_modB[40:48], key="g2s")
            if stop == 0:
                P.dma("sp", out[0:128, :], gB[:], r=[b_gB], key="dbg")
                P.dma("sp", out[128:256, 0:KD], A1[:], r=[b_A1], key="dbg1")
                P.dma("sp", out[128:256, KD:2 * KD], B1[:], r=[b_B1], key="dbg2")
                return nc, P.emit()
            P.barrier(barsc, [b_barp0])

        with contextlib.ExitStack() as st:
            pg = [PS(st, f"pg{i}", [128, 512], F32) for i in range(4)]
            ptb = [PS(st, f"ptb{i}", [128, 8, 128], BF16) for i in range(2)]
            py = PS(st, "py", [128, 1024], F32)
            b_pg = P.bufs(4, "pg", excl=True)
            b_ptb = P.bufs(2, "ptb", excl=True)
            b_py = P.buf("py", excl=True)
            barsc["p"] = pg[0]
            pgi = [0]

            def next_pg():
                i = pgi[0] % 4
                pgi[0] += 1
                return pg[i], b_pg[i]
            pti = [0]

            def next_pt():
                i = pti[0] % 2
                pti[0] += 1
                return ptb[i], b_ptb[i]

            if stop == 10:
                P.dma("sp", out[0:128, :], gB[:], r=[b_gB], key="dbg")
                return nc, P.emit()
            ut = SB(st, "ut", [128, 128], F32)
            ml = SB(st, "ml", [128, 128], F32)
            ones = SB(st, "ones", [128, 128], F32)
            cw = SB(st, "cw", [128, 32, 4], F32)
            cb = SB(st, "cb", [128, 32], F32)
            dtb = SB(st, "dtb", [128, NH], F32)
            aneg = SB(st, "aneg", [128, NH], F32)
            dsk = SB(st, "dsk", [128, NH], F32)
            sgBf = None
            sgB = SB(st, "sgB", [128, D_SSD], BF16)
            wpl = SB(st, "wpl", [128, 4, 2, 256], BF16)
            bps = SB(st, "bps", [128, 8], F32)
            psc = SB(st, "psc", [128, 8], F32)
            wr = SB(st, "wr", [128, KD, NE], F32)
            brB = SB(st, "brB", [128, NE], F32)
            lastv_t = SB(st, "lastv_t", [128, 1], F32)
            invc_t = SB(st, "invc_t", [128, 4, GT], F32)
            pmask_t = SB(st, "pmask_t", [128, max(TP, 128) // 128], F32)
            b_const = P.buf("const1")
            P.dma("sp", ut[:], utd[:, :], w=[b_const], key="c4")
            P.dma("sp", ml[:], mld[:, :], w=[b_const], key="c4")
            P.dma("sp", cw[:], conv_wT[:, :, :], w=[b_const], key="c4")
            P.dma("sp", cb[:], conv_bT[:, :], w=[b_const], key="c4")
            P.dma("sp", dtb[:], dt_bias[0:1, :].broadcast_to([128, NH]), w=[b_const], key="c4")
            P.dma("sp", aneg[:], a_log[0:1, :].broadcast_to([128, NH]), w=[b_const], key="c4")
            P.dma("sp", dsk[:], d_skip[0:1, :].broadcast_to([128, NH]), w=[b_const], key="c4")
            P.dma("sp", bps[:], b_poolT[:, :], w=[b_const], key="c4")
            P.dma("sp", psc[:], pscaleT[:, :], w=[b_const], key="c4")
            P.dma("sp", wr[:], w_router.rearrange("(kc p) e -> p kc e", p=128), w=[b_const], key="c4")
            P.dma("sp", brB[:], b_router[0:1, :].broadcast_to([128, NE]), w=[b_const], key="c4")
            P.dma("sp", lastv_t[:], lastv[:, :], w=[b_const], key="c4")
            P.dma("sp", invc_t[:], invc.rearrange("(o g) t -> o g t", o=1).broadcast_to([128, 4, GT]), w=[b_const], key="c4")
            P.dma("sp", pmask_t[:], pmask[:, :], w=[b_const], key="c4")
            b_c2 = P.buf("const2")
            P.op("dve", lambda h: h.memset(ones[:], 1.0), w=[b_c2])
            P.op("act", lambda h: h.activation(out=aneg[:], in_=aneg[:], func=AF.Exp), r=[b_const], w=[b_const])
            P.op("dve", lambda h: h.tensor_scalar(out=aneg[:], in0=aneg[:], scalar1=-1.0, scalar2=None, op0=ALU.mult), r=[b_const], w=[b_const])
            P.op("dve", lambda h: h.tensor_tensor(out=bps[:], in0=bps[:], in1=psc[:], op=ALU.mult), r=[b_const], w=[b_const])

            xst = SB(st, "xst", [128, D], F32)
            xn = SB(st, "xn", [128, D], BF16)
            ss = SB(st, "ss", [128, 4], F32)
            hT = SB(st, "hT", [128, KD, GT], BF16)
            wsl = [SB(st, f"wsl{i}", [128, 4096], BF16) for i in range(2)]
            zs = SB(st, "zs", [128, GT // 128, D_SSD], BF16)
            xbcT = SB(st, "xbcT", [128, 32, GT], BF16)
            pre = [SB(st, f"pre{i}", [128, GT + 3], F32) for i in range(2)]
            cacc = [SB(st, f"cacc{i}", [128, GT], F32) for i in range(2)]
            chalo = SB(st, "chalo", [128, 32, 3], F32)
            uT = SB(st, "uT", [128, 8, GT + 15], F32)
            ptmp = [SB(st, f"ptmp{i}", [128, GT + 15], F32) for i in range(2)]
            pooledT = SB(st, "pooledT", [128, 8, GT], BF16)
            yT = SB(st, "yT", [128, 32, GT], BF16)
            dtr = SB(st, "dtr", [128, GT // 128, NH], F32)
            dA = SB(st, "dA", [128, GT // 128, NH], F32)
            cum = SB(st, "cum", [128, NH], F32)
            ecum = SB(st, "ecum", [128, NH], F32)
            wend = SB(st, "wend", [128, NH], F32)
            etot = SB(st, "etot", [128, NH], F32)
            xs = SB(st, "xs", [128, D_SSD], BF16)
            btok = SB(st, "btok", [128, 512], BF16)
            xw = SB(st, "xw", [128, D_SSD], BF16)
            lt = [SB(st, f"lt{i}", [128, 128], F32) for i in range(3)]
            dec = [SB(st, f"dec{i}", [128, 128], F32) for i in range(3)]
            cbm = SB(st, "cbm", [128, 128], F32)
            mt = [SB(st, f"mt{i}", [128, 128], BF16) for i in range(3)]
            yg = SB(st, "yg", [128, 768], F32)
            ytmp = SB(st, "ytmp", [128, 768], F32)
            yn = SB(st, "yn", [128, 768], BF16)
            state = SB(st, "state", [128, D_SSD], F32)
            stbf = SB(st, "stbf", [128, D_SSD], BF16)
            xn2 = xst
            h2f = SB(st, "h2f", [128, KD, 128], F32)
            h2b = SB(st, "h2b", [128, KD, 128], BF16)
            lg = SB(st, "lg", [128, NE], F32)
            top8 = SB(st, "top8", [128, 8], F32)
            gsm = SB(st, "gsm", [128, 4], F32)

            b_sgB = P.buf("sgB")
            b_wpl = P.buf("wpl")
            b_xst0 = P.buf("xst0")
            wpl_f = xst[:].rearrange("p (g c d) -> p g c d", g=4, c=2)
            P.dma("sp", wpl_f, w_pool.rearrange("g (c2 p) d -> p g c2 d", p=128), w=[b_xst0], key="xst0")
            P.op("dve", lambda h: h.tensor_copy(out=wpl[:], in_=wpl_f), r=[b_xst0], w=[b_wpl])
            P.dma("sp", xst[:, 0:1536].bitcast(F32) if False else xst[:, 0:D], ssd_g[0:1, 0:D].broadcast_to([128, D]), r=[b_wpl], w=[b_xst0], key="xst0")
            P.op("dve", lambda h: h.tensor_copy(out=sgB[:, 0:D], in_=xst[:, 0:D]), r=[b_xst0], w=[b_sgB])
            P.dma("sp", xst[:, 0:D_SSD - D], ssd_g[0:1, D:D_SSD].broadcast_to([128, D_SSD - D]), r=[b_sgB], w=[b_xst0], key="xst0")
            P.op("dve", lambda h: h.tensor_copy(out=sgB[:, D:D_SSD], in_=xst[:, 0:D_SSD - D]), r=[b_xst0], w=[b_sgB])
            (b_xst, b_xn, b_ss, b_hT, b_zs, b_chalo, b_uT, b_pooledT, b_dtr, b_dA, b_cum, b_ecum, b_wend, b_etot,
             b_xs_, b_btok, b_xw, b_cbm, b_yg, b_ytmp, b_yn, b_x1t, b_xn2_, b_h2f, b_h2b, b_lg, b_top8, b_gsm) = P.bufs(28, "w")
            b_xn2 = b_xst
            b_hT = P.bufs(2, "hT")
            b_xs = P.bufs(3, "xs")
            b_wsl = P.bufs(2, "wsl")
            b_xbcT = P.bufs(32, "xbcT")
            b_pre = P.bufs(2, "pre")
            b_cacc = P.bufs(2, "cacc")
            b_ptmp = P.bufs(2, "ptmp")
            b_yT = P.bufs(32, "yT")
            b_lt = P.bufs(3, "lt")
            b_dec = P.bufs(3, "dec")
            b_mt = P.bufs(3, "mt")
            b_state = P.bufs(NG, "state")
            b_stbf = P.bufs(NG, "stbf")

            if stop == 11:
                P.dma("sp", out[0:128, :], gB[:], r=[b_gB, b_const, b_c2, b_sgB, b_wpl], key="dbg")
                return nc, P.emit()
            P.op("dve", lambda h: h.memset(state[:], 0.0), w=b_state)
            P.op("dve", lambda h: h.memset(stbf[:], 0.0), w=b_stbf)
            P.op("dve", lambda h: h.memset(chalo[:], 0.0), w=[b_chalo])
            P.op("dve", lambda h: h.memset(uT[:], 0.0), w=[b_uT])
            wsi = [0]

            def load_slab(src_ap, view, dims):
                i = wsi[0] % 2
                wsi[0] += 1
                n = 1
                for d_ in dims:
                    n *= d_
                dst = wsl[i][:, 0:n]
                if len(dims) == 2:
                    dst = dst.rearrange("p (a b) -> p a b", a=dims[0])
                P.dma("pool", dst, src_ap, w=[b_wsl[i]], key=f"wsl{i}")
                return dst, b_wsl[i]

            def rms_front(src_rows, sub, Acol, Bcol, dst_hT, b_dst):
                P.dma("sp", xst[:], src_rows, r=[b_sgB], w=[b_xst], key="xst")
                P.op("act", lambda h: h.activation(out=xn[:], in_=xst[:], func=AF.Square, accum_out=ss[:, 0:1]),
                     r=[b_xst], w=[b_xn, b_ss])
                P.op("act", lambda h: h.activation(out=ss[:, 1:2], in_=ss[:, 0:1], func=AF.Sqrt, scale=1.0 / D, bias=EPS),
                     r=[b_ss], w=[b_ss])
                P.op("dve", lambda h: h.reciprocal(out=ss[:, 2:3], in_=ss[:, 1:2]), r=[b_ss], w=[b_ss])
                P.op("act", lambda h: h.activation(out=xn[:], in_=xst[:], func=AF.Copy, scale=ss[:, 2:3]),
                     r=[b_xst, b_ss], w=[b_xn])
                for half in range(2):
                    pt, bpt = next_pt()
                    for j in range(8):
                        kc = half * 8 + j
                        P.op("pe", lambda h, pt=pt, j=j, kc=kc: h.transpose(out=pt[:, j, :], in_=xn[:, kc * 128:(kc + 1) * 128], identity=identb[:]),
                             r=[b_xn, b_identb], w=[bpt])
                    for j in range(8):
                        kc = half * 8 + j
                        eng = "act" if half == 0 else "dve"
                        if eng == "act":
                            P.op("act", lambda h, pt=pt, j=j, kc=kc: h.activation(out=dst_hT[:, kc, sub * 128:(sub + 1) * 128], in_=pt[:, j, :],
                                                                                  func=AF.Identity, scale=Acol[:, kc:kc + 1], bias=Bcol[:, kc:kc + 1]),
                                 r=[bpt, b_A1, b_B1, b_A2, b_B2], w=[b_dst[0]])
                        else:
                            P.op("dve", lambda h, pt=pt, j=j, kc=kc: h.tensor_scalar(out=dst_hT[:, kc, sub * 128:(sub + 1) * 128], in0=pt[:, j, :],
                                                                                     scalar1=Acol[:, kc:kc + 1], scalar2=Bcol[:, kc:kc + 1], op0=ALU.mult, op1=ALU.add),
                                 r=[bpt, b_A1, b_B1, b_A2, b_B2], w=[b_dst[1]])

            def group(src, tok0, prefix, first_own, need_u, maskcol0):
                NS = GT // 128
                for sub in range(NS):
                    rms_front(src[tok0 + sub * 128: tok0 + (sub + 1) * 128, :], sub, A1, B1, hT, b_hT)
                if lvl == 1:
                    return
                if not prefix:
                    for s_ in range(12):
                        c0 = s_ * 256
                        sl, bsl = load_slab(w_in[:, c0:c0 + 256].rearrange("(kc p) c -> p kc c", p=128), None, (KD, 256))
                        for sub in range(NS):
                            pz, bpz = next_pg()
                            for kc in range(KD):
                                P.op("pe", lambda h, pz=pz, sl=sl, kc=kc, sub=sub: h.matmul(pz[:, 0:256], lhsT=hT[:, kc, sub * 128:(sub + 1) * 128], rhs=sl[:, kc, :],
                                                                                            start=(kc == 0), stop=(kc == KD - 1)),
                                     r=b_hT + [bsl], w=[bpz])
                            P.op("act", lambda h, pz=pz, sub=sub, c0=c0: h.activation(out=zs[:, sub, c0:c0 + 256], in_=pz[:, 0:256], func=AF.Silu),
                                 r=[bpz], w=[b_zs])
                if lvl == 2:
                    return
                for s_ in range(16):
                    if prefix and (not need_u) and s_ >= 14:
                        continue
                    c0 = OFF_XBC + s_ * 256
                    sl, bsl = load_slab(w_in[:, c0:c0 + 256].rearrange("(kc p) c -> p kc c", p=128), None, (KD, 256))
                    for q in range(2):
                        cc = s_ * 2 + q
                        px, bpx = next_pg()
                        for kc in range(KD):
                            P.op("pe", lambda h, px=px, sl=sl, kc=kc, q=q: h.matmul(px[:, 0:GT], lhsT=sl[:, kc, q * 128:(q + 1) * 128], rhs=hT[:, kc, :],
                                                                                    start=(kc == 0), stop=(kc == KD - 1)),
                                 r=b_hT + [bsl], w=[bpx])
                        i = cc % 2
                        P.op("dve", lambda h, i=i, cc=cc: h.tensor_copy(out=pre[i][:, 0:3], in_=chalo[:, cc, :]), r=[b_chalo], w=[b_pre[i]])
                        P.op("act", lambda h, i=i, px=px: h.activation(out=pre[i][:, 3:3 + GT], in_=px[:, 0:GT], func=AF.Copy), r=[bpx], w=[b_pre[i]])
                        P.op("dve", lambda h, i=i, cc=cc: h.tensor_copy(out=chalo[:, cc, :], in_=pre[i][:, GT:GT + 3]), r=[b_pre[i]], w=[b_chalo])
                        P.op("dve", lambda h, i=i, cc=cc: h.tensor_scalar(out=cacc[i][:], in0=pre[i][:, 0:GT], scalar1=cw[:, cc, 0:1], scalar2=None, op0=ALU.mult),
                             r=[b_pre[i], b_const], w=[b_cacc[i]])
                        for k in (1, 2, 3):
                            P.op("dve", lambda h, i=i, cc=cc, k=k: h.scalar_tensor_tensor(out=cacc[i][:], in0=pre[i][:, k:k + GT], scalar=cw[:, cc, k:k + 1], in1=cacc[i][:],
                                                                                          op0=ALU.mult, op1=ALU.add),
                                 r=[b_pre[i], b_const, b_cacc[i]], w=[b_cacc[i]])
                        P.op("act", lambda h, i=i, cc=cc: h.activation(out=xbcT[:, cc, :], in_=cacc[i][:], func=AF.Silu, bias=cb[:, cc:cc + 1]),
                             r=[b_cacc[i], b_const], w=[b_xbcT[cc]])
                if lvl == 3:
                    return
                sl, bsl = load_slab(w_in[:, OFF_DT:OFF_DT + NH].rearrange("(kc p) c -> p kc c", p=128), None, (KD, NH))
                for sub in range(NS):
                    pd, bpd = next_pg()
                    for kc in range(KD):
                        P.op("pe", lambda h, pd=pd, sl=sl, kc=kc, sub=sub: h.matmul(pd[:, 0:NH], lhsT=hT[:, kc, sub * 128:(sub + 1) * 128], rhs=sl[:, kc, :],
                                                                                    start=(kc == 0), stop=(kc == KD - 1)),
                             r=b_hT + [bsl], w=[bpd])
                    P.op("dve", lambda h, pd=pd, sub=sub: h.tensor_tensor(out=dtr[:, sub, :], in0=pd[:, 0:NH], in1=dtb[:], op=ALU.add), r=[bpd, b_const], w=[b_dtr])
                    P.op("act", lambda h, sub=sub: h.activation(out=dtr[:, sub, :], in_=dtr[:, sub, :], func=AF.Exp), r=[b_dtr], w=[b_dtr])
                    P.op("act", lambda h, sub=sub: h.activation(out=dtr[:, sub, :], in_=dtr[:, sub, :], func=AF.Ln, bias=1.0), r=[b_dtr], w=[b_dtr])
                    if prefix:
                        mc = maskcol0 + sub
                        P.op("dve", lambda h, sub=sub, mc=mc: h.tensor_scalar(out=dtr[:, sub, :], in0=dtr[:, sub, :], scalar1=pmask_t[:, mc:mc + 1], scalar2=None, op0=ALU.mult),
                             r=[b_dtr, b_const], w=[b_dtr])
                    P.op("dve", lambda h, sub=sub: h.tensor_tensor(out=dA[:, sub, :], in0=dtr[:, sub, :], in1=aneg[:], op=ALU.mult), r=[b_dtr, b_const], w=[b_dA])
                if lvl == 4:
                    return
                if need_u:
                    for s_ in range(4):
                        c0 = OFF_U + s_ * 256
                        sl, bsl = load_slab(w_in[:, c0:c0 + 256].rearrange("(kc p) c -> p kc c", p=128), None, (KD, 256))
                        for q in range(2):
                            cc = s_ * 2 + q
                            pu, bpu = next_pg()
                            for kc in range(KD):
                                P.op("pe", lambda h, pu=pu, sl=sl, kc=kc, q=q: h.matmul(pu[:, 0:GT], lhsT=sl[:, kc, q * 128:(q + 1) * 128], rhs=hT[:, kc, :],
                                                                                        start=(kc == 0), stop=(kc == KD - 1)),
                                     r=b_hT + [bsl], w=[bpu])
                            P.op("act", lambda h, pu=pu, cc=cc: h.activation(out=uT[:, cc, 15:15 + GT], in_=pu[:, 0:GT], func=AF.Copy), r=[bpu], w=[b_uT])
                if need_u and not prefix:
                    L = GT + 15
                    for gi in range(4):
                        wlen = 2 << gi
                        for c2 in range(2):
                            cc = 2 * gi + c2
                            cur = uT[:, cc, :]
                            bcur = b_uT
                            sh = 1
                            lo = 0
                            for step in range(gi + 1):
                                i = step % 2
                                lo2 = lo + sh
                                P.op("dve", lambda h, cur=cur, i=i, lo2=lo2, sh=sh: h.tensor_tensor(out=ptmp[i][:, lo2:L], in0=cur[:, lo2:L], in1=cur[:, lo2 - sh:L - sh], op=ALU.add),
                                     r=[bcur], w=[b_ptmp[i]])
                                cur = ptmp[i]
                                bcur = b_ptmp[i]
                                lo = lo2
                                sh *= 2
                            if first_own:
                                P.op("dve", lambda h, cur=cur, gi=gi: h.tensor_tensor(out=cur[:, 15:L], in0=cur[:, 15:L], in1=invc_t[:, gi, :], op=ALU.mult),
                                     r=[bcur, b_const], w=[bcur])
                                P.op("dve", lambda h, cur=cur, cc=cc: h.tensor_tensor(out=pooledT[:, cc, :], in0=cur[:, 15:L], in1=uT[:, cc, 15:L], op=ALU.subtract),
                                     r=[bcur, b_uT], w=[b_pooledT])
                            else:
                                P.op("dve", lambda h, cur=cur, cc=cc, wlen=wlen: h.scalar_tensor_tensor(out=pooledT[:, cc, :], in0=cur[:, 15:L], scalar=1.0 / wlen, in1=uT[:, cc, 15:L],
                                                                                                        op0=ALU.mult, op1=ALU.subtract),
                                     r=[bcur, b_uT], w=[b_pooledT])
                    for gi in range(4):
                        for dc in range(2):
                            pp, bpp = next_pg()
                            for c2 in range(2):
                                P.op("pe", lambda h, pp=pp, gi=gi, dc=dc, c2=c2: h.matmul(pp[:, 0:GT], lhsT=wpl[:, gi, c2, dc * 128:(dc + 1) * 128], rhs=pooledT[:, 2 * gi + c2, :],
                                                                                          start=(c2 == 0), stop=(c2 == 1)),
                                     r=[b_pooledT, b_wpl], w=[bpp])
                            cc = 2 * gi + dc
                            P.op("act", lambda h, pp=pp, cc=cc: h.activation(out=yT[:, 24 + cc, :], in_=pp[:, 0:GT], func=AF.Identity, scale=psc[:, cc:cc + 1], bias=bps[:, cc:cc + 1]),
                                 r=[bpp, b_const], w=[b_yT[24 + cc]])
                if need_u:
                    P.op("dve", lambda h: h.tensor_copy(out=ptmp[0][:, 0:15 * 8].rearrange("p (c t) -> p c t", c=8), in_=uT[:, :, GT:GT + 15]), r=[b_uT], w=[b_ptmp[0]])
                    P.op("dve", lambda h: h.tensor_copy(out=uT[:, :, 0:15], in_=ptmp[0][:, 0:15 * 8].rearrange("p (c t) -> p c t", c=8)), r=[b_ptmp[0]], w=[b_uT])
                if lvl == 5:
                    return
                for sub in range(NS):
                    t0 = sub * 128
                    for blk in range(3):
                        pt, bpt = next_pt()
                        for j in range(8):
                            cc = blk * 8 + j
                            P.op("pe", lambda h, pt=pt, j=j, cc=cc, t0=t0: h.transpose(out=pt[:, j, :], in_=xbcT[:, cc, t0:t0 + 128], identity=identb[:]),
                                 r=[b_xbcT[cc], b_identb], w=[bpt])
                        eng = "act" if blk % 2 == 0 else "dve"
                        if eng == "act":
                            P.op("act", lambda h, pt=pt, blk=blk: h.activation(out=xs[:, blk * 1024:(blk + 1) * 1024], in_=pt[:].rearrange("p a b -> p (a b)"), func=AF.Copy),
                                 r=[bpt], w=[b_xs[blk]])
                        else:
                            P.op("dve", lambda h, pt=pt, blk=blk: h.tensor_copy(out=xs[:, blk * 1024:(blk + 1) * 1024], in_=pt[:].rearrange("p a b -> p (a b)")),
                                 r=[bpt], w=[b_xs[blk]])
                    pt, bpt = next_pt()
                    for j in range(4):
                        cc = 24 + j
                        P.op("pe", lambda h, pt=pt, j=j, cc=cc, t0=t0: h.transpose(out=pt[:, j, :], in_=xbcT[:, cc, t0:t0 + 128], identity=identb[:]),
                             r=[b_xbcT[cc], b_identb], w=[bpt])
                    P.op("dve", lambda h, pt=pt: h.tensor_copy(out=btok[:], in_=pt[:, 0:4, :].rearrange("p a b -> p (a b)")), r=[bpt], w=[b_btok])
                    pc, bpc = next_pg()
                    P.op("pe", lambda h, pc=pc, sub=sub: h.matmul(pc[:, 0:NH], lhsT=ut[:], rhs=dA[:, sub, :], start=True, stop=True), r=[b_dA, b_const], w=[bpc])
                    P.op("pe", lambda h, pc=pc, sub=sub: h.matmul(pc[:, 64:64 + NH], lhsT=ones[:], rhs=dA[:, sub, :], start=True, stop=True), r=[b_dA, b_c2], w=[bpc])
                    P.op("act", lambda h, pc=pc: h.activation(out=cum[:], in_=pc[:, 0:NH], func=AF.Copy), r=[bpc], w=[b_cum])
                    P.op("act", lambda h, pc=pc: h.activation(out=etot[:], in_=pc[:, 64:64 + NH], func=AF.Exp), r=[bpc], w=[b_etot])
                    P.op("dve", lambda h, pc=pc: h.tensor_tensor(out=wend[:], in0=pc[:, 64:64 + NH], in1=cum[:], op=ALU.subtract), r=[bpc, b_cum], w=[b_wend])
                    P.op("act", lambda h: h.activation(out=wend[:], in_=wend[:], func=AF.Exp), r=[b_wend], w=[b_wend])
                    P.op("dve", lambda h, sub=sub: h.tensor_tensor(out=wend[:], in0=wend[:], in1=dtr[:, sub, :], op=ALU.mult), r=[b_wend, b_dtr], w=[b_wend])
                    if not prefix:
                        P.op("act", lambda h: h.activation(out=ecum[:], in_=cum[:], func=AF.Exp), r=[b_cum], w=[b_ecum])
                    for g in range(NG):
                        gs = slice(g * 768, (g + 1) * 768)
                        hs = slice(g * HPG, (g + 1) * HPG)
                        if not prefix:
                            pcb, bpcb = next_pg()
                            P.op("pe", lambda h, pcb=pcb, g=g, t0=t0: h.matmul(pcb[:, 0:128], lhsT=xbcT[:, 24 + g, t0:t0 + 128], rhs=xbcT[:, 28 + g, t0:t0 + 128], start=True, stop=True),
                                 r=[b_xbcT[24 + g], b_xbcT[28 + g]], w=[bpcb])
                            P.op("dve", lambda h, pcb=pcb: h.tensor_tensor(out=cbm[:], in0=pcb[:, 0:128], in1=ut[:], op=ALU.mult), r=[bpcb, b_const], w=[b_cbm])
                            for r_ in range(HPG):
                                hh = g * HPG + r_
                                i = hh % 3
                                P.op("dve", lambda h, i=i, hh=hh, sub=sub: h.tensor_scalar(out=lt[i][:], in0=ml[:], scalar1=dA[:, sub, hh:hh + 1], scalar2=None, op0=ALU.mult),
                                     r=[b_dA, b_const], w=[b_lt[i]])
                                psg, bpsg = next_pg()
                                P.op("pe", lambda h, psg=psg, i=i: h.matmul(psg[:, 0:128], lhsT=lt[i][:], rhs=ut[:], start=True, stop=True), r=[b_lt[i], b_const], w=[bpsg])
                                P.op("act", lambda h, psg=psg, i=i: h.activation(out=dec[i][:], in_=psg[:, 0:128], func=AF.Exp), r=[bpsg], w=[b_dec[i]])
                                P.op("dve", lambda h, i=i, hh=hh, sub=sub: h.scalar_tensor_tensor(out=mt[i][:], in0=dec[i][:], scalar=dtr[:, sub, hh:hh + 1], in1=cbm[:], op0=ALU.mult, op1=ALU.mult),
                                     r=[b_dec[i], b_dtr, b_cbm], w=[b_mt[i]])
                                P.op("pe", lambda h, i=i, hh=hh, r_=r_: h.matmul(py[:, r_ * 64:(r_ + 1) * 64], lhsT=mt[i][:], rhs=xs[:, hh * 64:(hh + 1) * 64], start=True, stop=True),
                                     r=[b_mt[i]] + b_xs, w=[b_py])
                            po1, bpo1 = next_pg()
                            po2, bpo2 = next_pg()
                            P.op("pe", lambda h, po1=po1, g=g, t0=t0: h.matmul(po1[:, 0:512], lhsT=xbcT[:, 28 + g, t0:t0 + 128], rhs=stbf[:, g * 768:g * 768 + 512], start=True, stop=True),
                                 r=[b_xbcT[28 + g], b_stbf[g]], w=[bpo1])
                            P.op("pe", lambda h, po2=po2, g=g, t0=t0: h.matmul(po2[:, 0:256], lhsT=xbcT[:, 28 + g, t0:t0 + 128], rhs=stbf[:, g * 768 + 512:(g + 1) * 768], start=True, stop=True),
                                 r=[b_xbcT[28 + g], b_stbf[g]], w=[bpo2])
                            P.op("dve", lambda h, po1=po1, g=g: h.tensor_tensor(out=ytmp[:, 0:512].rearrange("p (a b) -> p a b", b=64), in0=po1[:, 0:512].rearrange("p (a b) -> p a b", b=64),
                                                                                in1=ecum[:, g * HPG:g * HPG + 8].unsqueeze(2).broadcast_to([128, 8, 64]), op=ALU.mult),
                                 r=[bpo1, b_ecum], w=[b_ytmp])
                            P.op("dve", lambda h, po2=po2, g=g: h.tensor_tensor(out=ytmp[:, 512:768].rearrange("p (a b) -> p a b", b=64), in0=po2[:, 0:256].rearrange("p (a b) -> p a b", b=64),
                                                                                in1=ecum[:, g * HPG + 8:(g + 1) * HPG].unsqueeze(2).broadcast_to([128, 4, 64]), op=ALU.mult),
                                 r=[bpo2, b_ecum], w=[b_ytmp])
                            P.op("dve", lambda h: h.tensor_tensor(out=yg[:], in0=py[:, 0:768], in1=ytmp[:], op=ALU.add), r=[b_py, b_ytmp], w=[b_yg])
                            P.op("dve", lambda h, gs=gs, hs=hs: h.tensor_tensor(out=ytmp[:].rearrange("p (a b) -> p a b", b=64), in0=xs[:, gs].rearrange("p (a b) -> p a b", b=64),
                                                                                 in1=dsk[:, hs].unsqueeze(2).broadcast_to([128, HPG, 64]), op=ALU.mult),
                                 r=b_xs + [b_const], w=[b_ytmp])
                            P.op("dve", lambda h: h.tensor_tensor(out=yg[:], in0=yg[:], in1=ytmp[:], op=ALU.add), r=[b_yg, b_ytmp], w=[b_yg])
                            P.op("dve", lambda h, sub=sub, gs=gs: h.tensor_tensor(out=yg[:], in0=yg[:], in1=zs[:, sub, gs], op=ALU.mult), r=[b_yg, b_zs], w=[b_yg])
                            P.op("act", lambda h: h.activation(out=ytmp[:], in_=yg[:], func=AF.Square, accum_out=gsm[:, 0:1]), r=[b_yg], w=[b_ytmp, b_gsm])
                            P.op("act", lambda h: h.activation(out=gsm[:, 1:2], in_=gsm[:, 0:1], func=AF.Sqrt, scale=1.0 / 768, bias=EPS), r=[b_gsm], w=[b_gsm])
                            P.op("dve", lambda h: h.reciprocal(out=gsm[:, 2:3], in_=gsm[:, 1:2]), r=[b_gsm], w=[b_gsm])
                            P.op("dve", lambda h, gs=gs: h.scalar_tensor_tensor(out=yn[:], in0=yg[:], scalar=gsm[:, 2:3], in1=sgB[:, gs], op0=ALU.mult, op1=ALU.mult),
                                 r=[b_yg, b_gsm, b_sgB], w=[b_yn])
                            pt, bpt = next_pt()
                            for j in range(6):
                                P.op("pe", lambda h, pt=pt, j=j: h.transpose(out=pt[:, j, :], in_=yn[:, j * 128:(j + 1) * 128], identity=identb[:]), r=[b_yn, b_identb], w=[bpt])
                            for j in range(6):
                                cc = 6 * g + j
                                P.op("act" if g % 2 == 0 else "dve",
                                     (lambda h, pt=pt, j=j, cc=cc, t0=t0: h.activation(out=yT[:, cc, t0:t0 + 128], in_=pt[:, j, :], func=AF.Copy)) if g % 2 == 0 else
                                     (lambda h, pt=pt, j=j, cc=cc, t0=t0: h.tensor_copy(out=yT[:, cc, t0:t0 + 128], in_=pt[:, j, :])),
                                     r=[bpt], w=[b_yT[cc]])
                        P.op("dve", lambda h, gs=gs, hs=hs: h.tensor_tensor(out=xw[:, gs].rearrange("p (a b) -> p a b", b=64), in0=xs[:, gs].rearrange("p (a b) -> p a b", b=64),
                                                                             in1=wend[:, hs].unsqueeze(2).broadcast_to([128, HPG, 64]), op=ALU.mult),
                             r=b_xs + [b_wend], w=[b_xw])
                        pq1, bpq1 = next_pg()
                        pq2, bpq2 = next_pg()
                        P.op("pe", lambda h, pq1=pq1, g=g: h.matmul(pq1[:, 0:512], lhsT=btok[:, g * 128:(g + 1) * 128], rhs=xw[:, g * 768:g * 768 + 512], start=True, stop=True),
                             r=[b_btok, b_xw], w=[bpq1])
                        P.op("pe", lambda h, pq2=pq2, g=g: h.matmul(pq2[:, 0:256], lhsT=btok[:, g * 128:(g + 1) * 128], rhs=xw[:, g * 768 + 512:(g + 1) * 768], start=True, stop=True),
                             r=[b_btok, b_xw], w=[bpq2])
                        P.op("dve", lambda h, gs=gs, hs=hs: h.tensor_tensor(out=state[:, gs].rearrange("p (a b) -> p a b", b=64), in0=state[:, gs].rearrange("p (a b) -> p a b", b=64),
                                                                            in1=etot[:, hs].unsqueeze(2).broadcast_to([128, HPG, 64]), op=ALU.mult),
                             r=[b_state[g], b_etot], w=[b_state[g]])
                        P.op("dve", lambda h, pq1=pq1, g=g: h.tensor_tensor(out=state[:, g * 768:g * 768 + 512], in0=state[:, g * 768:g * 768 + 512], in1=pq1[:, 0:512], op=ALU.add),
                             r=[b_state[g], bpq1], w=[b_state[g]])
                        P.op("dve", lambda h, pq2=pq2, g=g: h.tensor_tensor(out=state[:, g * 768 + 512:(g + 1) * 768], in0=state[:, g * 768 + 512:(g + 1) * 768], in1=pq2[:, 0:256], op=ALU.add),
                             r=[b_state[g], bpq2], w=[b_state[g]])
                        P.op("act", lambda h, gs=gs: h.activation(out=stbf[:, gs], in_=state[:, gs], func=AF.Copy), r=[b_state[g]], w=[b_stbf[g]])
                if prefix:
                    return
                if lvl == 6:
                    return
                x1acc = {}
                for sub in range(NS):
                    pass
                for os_ in range(16):
                    c0 = os_ * 128
                    sl, bsl = load_slab(w_out[:, c0:c0 + 128].rearrange("(cc p) c -> p cc c", p=128), None, (32, 128))
                    for sub in range(NS):
                        po, bpo = next_pg()
                        for cc in range(32):
                            P.op("pe", lambda h, po=po, sl=sl, cc=cc, sub=sub: h.matmul(po[:, 0:128], lhsT=yT[:, cc, sub * 128:(sub + 1) * 128], rhs=sl[:, cc, :],
                                                                                        start=(cc == 0), stop=(cc == 31)),
                                 r=[b_yT[cc], bsl], w=[bpo])
                        P.op("dve", lambda h, po=po, c0=c0, sub=sub: h.tensor_tensor(out=x1g[sub][:, c0:c0 + 128], in0=po[:, 0:128], in1=gB[:, c0:c0 + 128], op=ALU.mult),
                             r=[bpo, b_gB], w=[b_x1g[sub]])
                for sub in range(NS):
                    tok = tok0 + sub * 128
                    tile_i = tok // 128
                    P.dma("sp", xst[:], src[tok:tok + 128, :], w=[b_xst], key="xst")
                    P.op("dve", lambda h, sub=sub: h.tensor_tensor(out=x1g[sub][:], in0=x1g[sub][:], in1=xst[:], op=ALU.add), r=[b_x1g[sub], b_xst], w=[b_x1g[sub]])
                    P.dma("sp", x1_scr[tok:tok + 128, :], x1g[sub][:], r=[b_x1g[sub]], key=f"x1s{sub}")
                    if lvl == 7:
                        continue
                    P.op("act", lambda h, sub=sub: h.activation(out=xn2[:], in_=x1g[sub][:], func=AF.Square, accum_out=ss[:, 0:1]), r=[b_x1g[sub]], w=[b_xn2, b_ss])
                    P.op("act", lambda h: h.activation(out=ss[:, 1:2], in_=ss[:, 0:1], func=AF.Sqrt, scale=1.0 / D, bias=EPS), r=[b_ss], w=[b_ss])
                    P.op("dve", lambda h: h.reciprocal(out=ss[:, 2:3], in_=ss[:, 1:2]), r=[b_ss], w=[b_ss])
                    P.op("act", lambda h, sub=sub: h.activation(out=xn2[:], in_=x1g[sub][:], func=AF.Copy, scale=ss[:, 2:3]), r=[b_x1g[sub], b_ss], w=[b_xn2])
                    for q in range(4):
                        pf, bpf = next_pg()
                        for j in range(4):
                            kc = q * 4 + j
                            P.op("pe", lambda h, pf=pf, j=j, kc=kc: h.matmul(pf[:, j * 128:(j + 1) * 128], lhsT=xn2[:, kc * 128:(kc + 1) * 128], rhs=identf[:], start=True, stop=True),
                                 r=[b_xn2, b_identf], w=[bpf])
                        for j in range(4):
                            kc = q * 4 + j
                            P.op("act", lambda h, pf=pf, j=j, kc=kc: h.activation(out=h2f[:, kc, :], in_=pf[:, j * 128:(j + 1) * 128], func=AF.Identity,
                                                                                  scale=A2[:, kc:kc + 1], bias=B2[:, kc:kc + 1]),
                                 r=[bpf, b_A2, b_B2], w=[b_h2f])
                    P.op("dve", lambda h: h.tensor_copy(out=h2b[:], in_=h2f[:]), r=[b_h2f], w=[b_h2b])
                    P.dma("sp", h2T_scr[:, :, tok:tok + 128], h2b[:], r=[b_h2b], key="h2s")
                    if lvl == 8:
                        continue
                    pl, bpl = next_pg()
                    for kc in range(KD):
                        P.op("pe", lambda h, pl=pl, kc=kc: h.matmul(pl[:, 0:NE], lhsT=h2f[:, kc, :], rhs=wr[:, kc, :], start=(kc == 0), stop=(kc == KD - 1)),
                             r=[b_h2f, b_const], w=[bpl])
                    P.op("dve", lambda h, pl=pl: h.tensor_tensor(out=lg[:], in0=pl[:, 0:NE], in1=brB[:], op=ALU.add), r=[bpl, b_const], w=[b_lg])
                    P.op("dve", lambda h: h.max(out=top8[:], in_=lg[:]), r=[b_lg], w=[b_top8])
                    P.op("dve", lambda h: h.tensor_scalar(out=gsm[:, 3:4], in0=top8[:, 0:1], scalar1=-1.0, scalar2=None, op0=ALU.mult), r=[b_top8], w=[b_gsm])
                    gt_ = gates[:, tile_i, :]
                    bg_ = b_gates[tile_i]
                    P.op("act", lambda h, gt_=gt_: h.activation(out=gt_, in_=lg[:], func=AF.Exp, bias=gsm[:, 3:4]), r=[b_lg, b_gsm], w=[bg_])
                    P.op("dve", lambda h: h.tensor_scalar(out=lg[:], in0=lg[:], scalar1=top8[:, 3:4], scalar2=None, op0=ALU.is_ge), r=[b_lg, b_top8, bg_], w=[b_lg])
                    P.op("dve", lambda h, gt_=gt_: h.tensor_tensor(out=gt_, in0=gt_, in1=lg[:], op=ALU.mult), r=[bg_, b_lg], w=[bg_])
                    P.op("dve", lambda h, gt_=gt_: h.reduce_sum(out=gsm[:, 0:1], in_=gt_, axis=mybir.AxisListType.X), r=[bg_], w=[b_gsm])
                    P.op("dve", lambda h: h.reciprocal(out=gsm[:, 1:2], in_=gsm[:, 0:1]), r=[b_gsm], w=[b_gsm])
                    P.op("dve", lambda h, gt_=gt_: h.tensor_scalar(out=gt_, in0=gt_, scalar1=gsm[:, 1:2], scalar2=None, op0=ALU.mult), r=[bg_, b_gsm], w=[bg_])

            x1g = [SB(st, f"x1g{i}", [128, D], F32) for i in range(GT // 128)]
            b_x1g = P.bufs(GT // 128, "x1g")

            ngp = TP // GT
            for gi_ in range(ngp):
                last = (gi_ == ngp - 1)
                group(x_prev, gi_ * GT, True, False, last, gi_ * (GT // 128))
            if ngp > 0:
                P.op("dve", lambda h: h.tensor_scalar(out=chalo[:], in0=chalo[:], scalar1=lastv_t[:, 0:1], scalar2=None, op0=ALU.mult), r=[b_chalo, b_const], w=[b_chalo])
                P.op("dve", lambda h: h.tensor_scalar(out=uT[:, :, 0:15], in0=uT[:, :, 0:15], scalar1=lastv_t[:, 0:1], scalar2=None, op0=ALU.mult), r=[b_uT, b_const], w=[b_uT])
            for gi_ in range(T // GT):
                group(x_own, gi_ * GT, False, gi_ == 0, True, 0)
            if stop == 1:
                b_dd = P.buf("dd")
                for tt in range(T // 128):
                    P.dma("sp", xst[:], x1_scr[tt * 128:(tt + 1) * 128, :], w=[b_dd], r=[b_xst], key="dbg3")
                    P.dma("sp", out[tt * 128:(tt + 1) * 128, :], xst[:], r=[b_dd], key="dbg4")
                return nc, P.emit()
            P.barrier(barsc, [b_pg[0]])

        with contextlib.ExitStack() as st:
            NTP = PT // 128
            NHALF = PT // 512
            pgl = [PS(st, f"pgl{i}", [128, 512], F32) for i in range(4)]
            pov = [PS(st, f"pov{i}", [128, 512], F32) for i in range(3)]
            b_pgl = P.bufs(4, "pgl", excl=True)
            b_pov = P.bufs(3, "pov", excl=True)
            barsc["p"] = pgl[0]
            h2T = SB(st, "h2T", [128, KD, PT], BF16)
            acc = SB(st, "acc", [128, NTP, D], F32)
            actraw = SB(st, "actraw", [128, 16 * PT], BF16)
            actT = actraw[:].rearrange("p (f t) -> p f t", f=16)
            fngB = actraw[:, 0:2 * D].bitcast(F32)
            x1l = actraw[:, 2 * D:4 * D].bitcast(F32)
            wi = [SB(st, f"wi{i}", [128, KD, 256], BF16) for i in range(2)]
            wo = [SB(st, f"wo{i}", [128, 16, 512], BF16) for i in range(2)]
            beT = SB(st, "beT", [128, NE, 16, 2], F32)
            beL = SB(st, "beL", [128, NE, 16], F32)
            bo = actraw[0:NE, 6 * D:8 * D].bitcast(F32)
            gT = SB(st, "gT", [NE, 128], F32)
            tg = [SB(st, f"tg{i}", [128, 512], BF16) for i in range(2)]
            tsg = [SB(st, f"tsg{i}", [128, 512], BF16) for i in range(2)]
            tl = [SB(st, f"tl{i}", [128, 512], BF16) for i in range(2)]
            tt_ = [SB(st, f"tt{i}", [128, 512], BF16) for i in range(2)]
            ss2 = SB(st, "ss2", [128, 4], F32)
            b_h2T, b_beT, b_bo, b_gT, b_fngB, b_x1l, b_ss2, b_g2 = P.bufs(8, "m")
            b_acc = P.bufs(NTP, "acc")
            b_actT = P.bufs(16 * NHALF, "actT")
            b_wi = P.bufs(2, "wi")
            b_wo = P.bufs(2, "wo")
            b_tg = P.bufs(2, "tg")
            b_tsg = P.bufs(2, "tsg")
            b_tl = P.bufs(2, "tl")
            b_tt = P.bufs(2, "tt")
            P.dma("sp", beT[:], b_einT[:, :, :, :], w=[b_beT], key="m0")
            P.op("dve", lambda h: h.tensor_scalar(out=beL[:], in0=beT[:, :, :, 1], scalar1=1.0, scalar2=None, op0=ALU.add), r=[b_beT], w=[b_beT])
            P.dma("sp", gB[:], g2_scr[:, :], w=[b_g2], key="m3")
            cnt = dict(gl=0, ov=0, wi=0, wo=0, ep=0)
            for ps_ in range(T // PT):
                tokp = ps_ * PT
                P.dma("sp", h2T[:], h2T_scr[:, :, tokp:tokp + PT], w=[b_h2T], key="h2l")
                P.dma("sp", bo, b_eout[:, :], r=[b_x1l, b_fngB], w=[b_bo] + b_actT, key="m1")
                for tt in range(NTP):
                    tile_i = tokp // 128 + tt
                    pv = pov[cnt["ov"] % 3]; bpv = b_pov[cnt["ov"] % 3]; cnt["ov"] += 1
                    P.op("pe", lambda h, pv=pv, tile_i=tile_i: h.matmul(pv[0:NE, 0:128], lhsT=gates[:, tile_i, :], rhs=identf[:], start=True, stop=True),
                         r=[b_gates[tile_i], b_identf], w=[bpv])
                    P.op("act", lambda h, pv=pv: h.activation(out=gT[:], in_=pv[0:NE, 0:128], func=AF.Copy), r=[bpv], w=[b_gT])
                    for q in range(4):
                        pv2 = pov[cnt["ov"] % 3]; bpv2 = b_pov[cnt["ov"] % 3]; cnt["ov"] += 1
                        P.op("pe", lambda h, pv2=pv2, q=q: h.matmul(pv2[:, 0:512], lhsT=gT[:], rhs=bo[:, q * 512:(q + 1) * 512], start=True, stop=True),
                             r=[b_gT, b_bo], w=[bpv2])
                        P.op("act", lambda h, pv2=pv2, tt=tt, q=q: h.activation(out=acc[:, tt, q * 512:(q + 1) * 512], in_=pv2[:, 0:512], func=AF.Copy),
                             r=[bpv2], w=[b_acc[tt]])
                for e in range(NE):
                    for fc in range(16):
                        i = cnt["wi"] % 2; cnt["wi"] += 1
                        P.dma("pool", wi[i][:], w_ein[e, :, fc * 256:(fc + 1) * 256].rearrange("(kc p) c -> p kc c", p=128), w=[b_wi[i]], key=f"wi{i}")
                        wv = wi[i][:].rearrange("p k (f two) -> p k f two", two=2)
                        pgs = []
                        for hf in range(NHALF):
                            pgs.append((pgl[cnt["gl"] % 4], b_pgl[cnt["gl"] % 4])); cnt["gl"] += 1
                        for kc in range(KD):
                            for hf in range(NHALF):
                                pgt, bpg_ = pgs[hf]
                                P.op("pe", lambda h, pgt=pgt, wv=wv, kc=kc, hf=hf: h.matmul(pgt[:], lhsT=wv[:, kc, :, 0], rhs=h2T[:, kc, hf * 512:(hf + 1) * 512],
                                                                                           start=(kc == 0), stop=(kc == KD - 1)),
                                     r=[b_wi[i], b_h2T], w=[bpg_])
                        ks = []
                        for hf in range(NHALF):
                            pgt, bpg_ = pgs[hf]
                            k = cnt["ep"] % 2; cnt["ep"] += 1
                            ks.append(k)
                            P.op("dve", lambda h, pgt=pgt, k=k, e=e, fc=fc: h.tensor_scalar(out=tg[k][:], in0=pgt[:], scalar1=beT[:, e, fc, 0:1], scalar2=LIMIT, op0=ALU.add, op1=ALU.min),
                                 r=[bpg_, b_beT], w=[b_tg[k]])
                            P.op("act", lambda h, k=k: h.activation(out=tsg[k][:], in_=tg[k][:], func=AF.Sigmoid, scale=ALPHA), r=[b_tg[k]], w=[b_tsg[k]])
                            P.op("dve", lambda h, k=k: h.tensor_tensor(out=tt_[k][:], in0=tg[k][:], in1=tsg[k][:], op=ALU.mult), r=[b_tg[k], b_tsg[k]], w=[b_tt[k]])
                        pls = []
                        for hf in range(NHALF):
                            pls.append((pgl[cnt["gl"] % 4], b_pgl[cnt["gl"] % 4])); cnt["gl"] += 1
                        for kc in range(KD):
                            for hf in range(NHALF):
                                plt, bpl_ = pls[hf]
                                P.op("pe", lambda h, plt=plt, wv=wv, kc=kc, hf=hf: h.matmul(plt[:], lhsT=wv[:, kc, :, 1], rhs=h2T[:, kc, hf * 512:(hf + 1) * 512],
                                                                                           start=(kc == 0), stop=(kc == KD - 1)),
                                     r=[b_wi[i], b_h2T], w=[bpl_])
                        for hf in range(NHALF):
                            plt, bpl_ = pls[hf]
                            k = ks[hf]
                            P.op("dve", lambda h, plt=plt, k=k, e=e, fc=fc: h.tensor_scalar(out=tl[k][:], in0=plt[:], scalar1=beL[:, e, fc:fc + 1], scalar2=LIMIT + 1.0, op0=ALU.add, op1=ALU.min),
                                 r=[bpl_, b_beT], w=[b_tl[k]])
                            P.op("dve", lambda h, k=k, fc=fc, hf=hf: h.scalar_tensor_tensor(out=actT[:, fc, hf * 512:(hf + 1) * 512], in0=tl[k][:], scalar=1.0 - LIMIT, in1=tt_[k][:],
                                                                                            op0=ALU.max, op1=ALU.mult),
                                 r=[b_tt[k], b_tl[k]], w=[b_actT[fc * NHALF + hf], b_x1l, b_fngB, b_bo])
                    for os_ in range(4):
                        i = cnt["wo"] % 2; cnt["wo"] += 1
                        P.dma("pool", wo[i][:], w_eout[e, :, os_ * 512:(os_ + 1) * 512].rearrange("(fc p) c -> p fc c", p=128), w=[b_wo[i]], key=f"wo{i}")
                        for tt in range(NTP):
                            tile_i = tokp // 128 + tt
                            pv = pov[cnt["ov"] % 3]; bpv = b_pov[cnt["ov"] % 3]; cnt["ov"] += 1
                            hf = (tt * 128) // 512
                            for fc in range(16):
                                P.op("pe", lambda h, pv=pv, i=i, fc=fc, tt=tt: h.matmul(pv[:, 0:512], lhsT=actT[:, fc, tt * 128:(tt + 1) * 128], rhs=wo[i][:, fc, :],
                                                                                        start=(fc == 0), stop=(fc == 15)),
                                     r=[b_actT[fc * NHALF + hf], b_wo[i]], w=[bpv])
                            P.op("dve", lambda h, pv=pv, tt=tt, os_=os_, tile_i=tile_i, e=e: h.scalar_tensor_tensor(
                                out=acc[:, tt, os_ * 512:(os_ + 1) * 512], in0=pv[:, 0:512], scalar=gates[:, tile_i, e:e + 1],
                                in1=acc[:, tt, os_ * 512:(os_ + 1) * 512], op0=ALU.mult, op1=ALU.add),
                                 r=[bpv, b_gates[tile_i], b_acc[tt]], w=[b_acc[tt]])
                P.dma("sp", fngB, fng[0:1, :].broadcast_to([128, D]), r=b_actT, w=[b_fngB] + b_actT, key="m2")
                for tt in range(NTP):
                    tok = tokp + tt * 128
                    P.dma("sp", x1l, x1_scr[tok:tok + 128, :], r=b_actT, w=[b_x1l], key="x1l")
                    P.op("dve", lambda h, tt=tt: h.tensor_tensor(out=acc[:, tt, :], in0=acc[:, tt, :], in1=gB[:], op=ALU.mult), r=[b_acc[tt], b_g2], w=[b_acc[tt]])
                    P.op("dve", lambda h, tt=tt: h.tensor_tensor(out=x1l, in0=x1l, in1=acc[:, tt, :], op=ALU.add), r=[b_acc[tt], b_x1l], w=[b_x1l])
                    P.op("act", lambda h, tt=tt: h.activation(out=acc[:, tt, :], in_=x1l, func=AF.Square, accum_out=ss2[:, 0:1]), r=[b_x1l], w=[b_acc[tt], b_ss2])
                    P.op("act", lambda h: h.activation(out=ss2[:, 1:2], in_=ss2[:, 0:1], func=AF.Sqrt, scale=1.0 / D, bias=EPS), r=[b_ss2], w=[b_ss2])
                    P.op("dve", lambda h: h.reciprocal(out=ss2[:, 2:3], in_=ss2[:, 1:2]), r=[b_ss2], w=[b_ss2])
                    P.op("dve", lambda h, tt=tt: h.scalar_tensor_tensor(out=acc[:, tt, :], in0=x1l, scalar=ss2[:, 2:3], in1=fngB, op0=ALU.mult, op1=ALU.mult),
                         r=[b_x1l, b_ss2, b_fngB], w=[b_acc[tt]])
                    P.dma("sp", out[tok:tok + 128, :], acc[:, tt, :], r=[b_acc[tt]], key=f"outst{tt}")
            stats = P.emit()
    return nc, stats


def prep_inputs(cfg, inp):
    T, TP, NC, NE = cfg.T, cfg.TP, cfg.NC, cfg.NE
    f = np.float32
    x = np.ascontiguousarray(np.asarray(inp["x"], f)[0])
    colT = lambda v: np.ascontiguousarray(np.asarray(v, f).reshape(-1, 128).T)
    shared = dict(
        c_T=colT(inp["c"][0]),
        w_ada=np.asarray(inp["w_ada"], f)[0],
        b_ada=np.asarray(inp["b_ada"], f)[0][None, :],
        n1g_T=colT(inp["norm1_g"][0]),
        n2g_T=colT(inp["norm2_g"][0]),
        w_in=np.asarray(inp["w_in_proj"], f)[0],
        conv_wT=np.ascontiguousarray(np.asarray(inp["conv_w"], f)[0].T.reshape(32, 128, 4).transpose(1, 0, 2)),
        conv_bT=colT(inp["conv_b"][0]),
        dt_bias=np.asarray(inp["dt_bias"], f)[0][None, :],
        a_log=np.asarray(inp["a_log"], f)[0][None, :],
        d_skip=np.asarray(inp["d_skip"], f)[0][None, :],
        ssd_g=np.asarray(inp["ssd_norm_g"], f)[0][None, :],
        w_pool=np.asarray(inp["w_pool"], f)[0],
        b_poolT=colT(inp["b_pool"][0]),
        pscaleT=colT(inp["pool_scale"][0]),
        w_out=np.asarray(inp["w_out_proj"], f)[0],
        w_router=np.asarray(inp["w_router"], f)[0],
        b_router=np.asarray(inp["b_router"], f)[0][None, :],
        w_ein=np.asarray(inp["w_exp_in"], f)[0],
        b_einT=np.ascontiguousarray(np.asarray(inp["b_exp_in"], f)[0].reshape(NE, 16, 128, 2).transpose(2, 0, 1, 3)),
        w_eout=np.asarray(inp["w_exp_out"], f)[0],
        b_eout=np.asarray(inp["b_exp_out"], f)[0],
        fng=np.asarray(inp["final_norm_g"], f)[None, :],
        ident=np.eye(128, dtype=f),
        ut_c=np.triu(np.ones((128, 128), f)),
        ml_c=np.tril(np.ones((128, 128), f), -1),
    )
    maps = []
    TPp = max(TP, 128)
    for c in range(NC):
        m = dict(shared)
        m["x_own"] = x[c * T:(c + 1) * T]
        xp = np.zeros((TPp, D), f)
        pm = np.zeros((TPp,), f)
        nprev = c * T
        if nprev > 0:
            xp[TP - nprev:TP] = x[0:nprev]
            pm[TP - nprev:TP] = 1.0
        m["x_prev"] = xp
        m["pmask"] = np.ascontiguousarray(pm.reshape(-1, 128).T)
        m["lastv"] = np.full((128, 1), 1.0 if c > 0 else 0.0, f)
        ic = np.zeros((4, GT), f)
        for gi in range(4):
            w = 2 << gi
            tpos = np.arange(GT) + c * T + 1
            ic[gi] = 1.0 / np.minimum(tpos, w)
        m["invc"] = ic
        maps.append(m)
    return maps


_CACHE = {}


def run(cfg, inp):
    key = (cfg.SEQ, cfg.NC, cfg.NE, cfg.PT)
    if key not in _CACHE:
        _CACHE[key] = build_program(cfg)
    nc, stats = _CACHE[key]
    maps = prep_inputs(cfg, inp)
    res = run_bass_kernel_spmd(nc, maps, core_ids=list(range(cfg.NC)))
    outs = [np.asarray(r["out"]) for r in res.results]
    return np.concatenate(outs, axis=0)[None].astype(np.float32)


NCORES_FULL = 4
PT_FULL = 1024


def kernel(**inputs):
    cfg = Cfg(16384, NCORES_FULL, 32, PT_FULL)
    return run(cfg, inputs)
```

```python
import contextlib
import numpy as np
import concourse.bass as bass
import concourse.mybir as mybir
from concourse.bass_utils import run_bass_kernel_spmd

F32 = mybir.dt.float32
BF16 = mybir.dt.bfloat16
AF = mybir.ActivationFunctionType
ALU = mybir.AluOpType

D = 2048
KD = 16
D_SSD = 3072
NH = 48
HD = 64
NG = 4
HPG = 12
DST = 128
D_CONV = 4096
D_POOL = 1024
D_IN = 8240
OFF_XBC = 3072
OFF_DT = 7168
OFF_U = 7216
F_EXP = 2048
EPS = 1e-6
LIMIT = 7.0
ALPHA = 1.702
GT = 256

PHASE = 24000
SAME_ENGINE_RAW = True


class Buf:
    __slots__ = ("name", "lw", "rd", "excl")

    def __init__(self, name, excl=False):
        self.name = name
        self.excl = excl
        self.lw = None
        self.rd = []


class Op:
    __slots__ = ("eng", "fn", "is_dma", "key", "deps", "sig", "tok", "drain")

    def __init__(self, eng, fn, is_dma, key):
        self.eng = eng
        self.fn = fn
        self.is_dma = is_dma
        self.key = key
        self.deps = ()
        self.sig = False
        self.tok = None
        self.drain = False


class Prog:
    def __init__(self, nc, stack):
        self.nc = nc
        self.stack = stack
        self.ops = []
        self.h = {"pe": nc.tensor, "act": nc.scalar, "dve": nc.vector,
                  "pool": nc.gpsimd, "sp": nc.sync}
        self.nbuf = 0

    def buf(self, name=None, excl=False):
        self.nbuf += 1
        return Buf(name or f"b{self.nbuf}", excl)

    def bufs(self, n, name="b", excl=False):
        return [self.buf(f"{name}{i}", excl) for i in range(n)]

    def _add(self, op, r, w):
        i = len(self.ops)
        xr = [b for b in r if b.excl]
        if xr:
            w = list(w) + [b for b in xr if b not in w]
        raw = set()
        oth = set()
        for b in r:
            if b.lw is not None:
                raw.add(b.lw)
        for b in w:
            if b.lw is not None:
                oth.add(b.lw)
            for x in b.rd:
                oth.add(x)
        oth -= raw
        oth.discard(i)
        op.deps = tuple((d, True) for d in sorted(raw)) + tuple((d, False) for d in sorted(oth))
        self.ops.append(op)
        for b in r:
            b.rd.append(i)
        for b in w:
            b.lw = i
            b.rd = []
        return i

    def op(self, eng, fn, r=(), w=()):
        return self._add(Op(eng, fn, False, None), r, w)

    def dma(self, eng, out, in_, r=(), w=(), key=None, **kw):
        def fn(h):
            return h.dma_start(out=out, in_=in_, **kw)
        return self._add(Op(eng, fn, True, key or "dflt_" + eng), r, w)

    def barrier(self, scratch, pbuf=()):
        bs = {}
        for e in ("pe", "act", "dve", "pool", "sp"):
            bs[e] = self.buf("bar_" + e)
        sc = scratch
        zr = [sc["zbuf"]]
        self.op("act", lambda h: h.activation(out=sc["a"][:, 0:1], in_=sc["z"][:, 0:1], func=AF.Copy), r=zr, w=[bs["act"]])
        self.op("dve", lambda h: h.tensor_copy(out=sc["v"][:, 0:1], in_=sc["z"][:, 0:1]), r=zr, w=[bs["dve"]])
        self.op("pool", lambda h: h.tensor_copy(out=sc["g"][:, 0:1], in_=sc["z"][:, 0:1]), r=zr, w=[bs["pool"]])
        self.op("pe", lambda h: h.matmul(sc["p"][0:1, 0:1], lhsT=sc["zb"][:, 0:1], rhs=sc["zb"][:, 0:1], start=True, stop=True), r=zr, w=[bs["pe"]] + list(pbuf))
        i = self.op("sp", lambda h: h.nop(), w=[bs["sp"]])
        self.ops[i].drain = True
        allb = list(bs.values())
        self.op("act", lambda h: h.activation(out=sc["a"][:, 1:2], in_=sc["z"][:, 0:1], func=AF.Copy), r=allb)
        self.op("dve", lambda h: h.tensor_copy(out=sc["v"][:, 1:2], in_=sc["z"][:, 0:1]), r=allb)
        self.op("pool", lambda h: h.tensor_copy(out=sc["g"][:, 1:2], in_=sc["z"][:, 0:1]), r=allb)
        self.op("pe", lambda h: h.matmul(sc["p"][0:1, 1:2], lhsT=sc["zb"][:, 0:1], rhs=sc["zb"][:, 0:1], start=True, stop=True), r=allb, w=list(pbuf))
        self.op("sp", lambda h: h.nop(), r=allb)

    def emit(self, final_wait_eng="sp"):
        nc, ops = self.nc, self.ops
        for i, op in enumerate(ops):
            for d, israw in op.deps:
                p = ops[d]
                if p.is_dma:
                    p.sig = True
                elif p.eng != op.eng or op.is_dma:
                    p.sig = True
                elif SAME_ENGINE_RAW and p.eng != "pe":
                    p.sig = True
        for op in ops:
            if op.is_dma:
                op.sig = True
        sems = {}

        def getsem(name):
            if name not in sems:
                sems[name] = self.stack.enter_context(nc.semaphore(name))
            return sems[name]

        cnt = {}
        waited = {}
        n_wait = 0
        for i, op in enumerate(ops):
            h = self.h[op.eng]
            need = {}
            for d, israw in op.deps:
                p = ops[d]
                if p.tok is None:
                    continue
                if (not p.is_dma) and p.eng == op.eng and not op.is_dma:
                    if not (SAME_ENGINE_RAW and p.eng != "pe"):
                        continue
                sn, v = p.tok
                if need.get(sn, 0) < v:
                    need[sn] = v
            if op.drain:
                for sn in sems:
                    if sn.startswith("d_"):
                        need[sn] = max(need.get(sn, 0), cnt[sn])
            for sn, v in need.items():
                if waited.get((op.eng, sn), 0) >= v:
                    continue
                h.wait_ge(sems[sn], v)
                waited[(op.eng, sn)] = v
                n_wait += 1
            ins = op.fn(h)
            if op.sig:
                if op.is_dma:
                    sn = "d_" + op.key
                    inc = 16
                else:
                    base = "e_" + op.eng
                    ph = cnt.get(base + "_n", 0) // PHASE
                    cnt[base + "_n"] = cnt.get(base + "_n", 0) + 1
                    sn = f"{base}{ph}"
                    inc = 1
                s = getsem(sn)
                cnt[sn] = cnt.get(sn, 0) + inc
                ins.then_inc(s, inc)
                op.tok = (sn, cnt[sn])
        h = self.h[final_wait_eng]
        for sn, s in sems.items():
            if sn.startswith("d_"):
                if waited.get((final_wait_eng, sn), 0) < cnt[sn]:
                    h.wait_ge(s, cnt[sn])
        self.stats = dict(n_ops=len(ops), n_wait=n_wait, n_sems=len(sems))
        return self.stats


class Cfg:
    def __init__(self, seq, ncores, n_exp, pt=1024):
        self.SEQ = seq
        self.NC = ncores
        self.NE = n_exp
        self.T = seq // ncores
        self.TP = (ncores - 1) * self.T
        self.PT = min(pt, self.T)
        assert self.T % GT == 0 and self.T % self.PT == 0


def build_program(cfg, stop=9, lvl=9):
    T, TP, NE, PT = cfg.T, cfg.TP, cfg.NE, cfg.PT
    NTILE = T // 128
    nc = bass.Bass("TRN2", target_bir_lowering=False)

    def din(name, shape, dt=F32):
        return nc.dram_tensor(name, list(shape), dt, kind="ExternalInput").ap()

    x_own = din("x_own", [T, D])
    x_prev = din("x_prev", [max(TP, 128), D])
    pmask = din("pmask", [128, max(TP, 128) // 128])
    lastv = din("lastv", [128, 1])
    invc = din("invc", [4, GT])
    c_T = din("c_T", [128, KD])
    w_ada = din("w_ada", [D, 6 * D])
    b_ada = din("b_ada", [1, 6 * D])
    n1g_T = din("n1g_T", [128, KD])
    n2g_T = din("n2g_T", [128, KD])
    w_in = din("w_in", [D, D_IN])
    conv_wT = din("conv_wT", [128, 32, 4])
    conv_bT = din("conv_bT", [128, 32])
    dt_bias = din("dt_bias", [1, NH])
    a_log = din("a_log", [1, NH])
    d_skip = din("d_skip", [1, NH])
    ssd_g = din("ssd_g", [1, D_SSD])
    w_pool = din("w_pool", [4, 256, 256])
    b_poolT = din("b_poolT", [128, 8])
    pscaleT = din("pscaleT", [128, 8])
    w_out = din("w_out", [2 * D, D])
    w_router = din("w_router", [D, NE])
    b_router = din("b_router", [1, NE])
    w_ein = din("w_ein", [NE, D, 2 * F_EXP])
    b_einT = din("b_einT", [128, NE, 16, 2])
    w_eout = din("w_eout", [NE, F_EXP, D])
    b_eout = din("b_eout", [NE, D])
    fng = din("fng", [1, D])
    identd = din("ident", [128, 128])
    utd = din("ut_c", [128, 128])
    mld = din("ml_c", [128, 128])
    out = nc.dram_tensor("out", [T, D], F32, kind="ExternalOutput").ap()
    x1_scr = nc.dram_tensor("x1_scr", [T, D], F32).ap()
    h2T_scr = nc.dram_tensor("h2T_scr", [128, KD, T], BF16).ap()
    g2_scr = nc.dram_tensor("g2_scr", [128, D], F32).ap()

    with contextlib.ExitStack() as top:
        P = Prog(nc, top)

        def SB(st, name, shape, dt):
            return st.enter_context(nc.sbuf_tensor(name, list(shape), dt))

        def PS(st, name, shape, dt):
            return st.enter_context(nc.psum_tensor(name, list(shape), dt))

        identf = SB(top, "identf", [128, 128], F32)
        identb = SB(top, "identb", [128, 128], BF16)
        A1 = SB(top, "A1", [128, KD], F32)
        B1 = SB(top, "B1", [128, KD], F32)
        A2 = SB(top, "A2", [128, KD], F32)
        B2 = SB(top, "B2", [128, KD], F32)
        gB = SB(top, "gB", [128, D], F32)
        gates = SB(top, "gates", [128, NTILE, NE], F32)
        barsc = dict(a=SB(top, "bar_a", [128, 2], F32), v=SB(top, "bar_v", [128, 2], F32),
                     g=SB(top, "bar_g", [128, 2], F32), z=SB(top, "bar_z", [128, 2], F32),
                     zb=SB(top, "bar_zb", [128, 2], BF16))
        b_identf, b_identb, b_A1, b_B1, b_A2, b_B2, b_gB, b_barz = P.bufs(8, "pers")
        b_gates = P.bufs(NTILE, "gates")
        P.dma("sp", identf[:], identd[:, :], w=[b_identf], key="c0")
        P.op("dve", lambda h: h.tensor_copy(out=identb[:], in_=identf[:]), r=[b_identf], w=[b_identb])
        P.op("dve", lambda h: h.memset(barsc["z"][:], 0.0), w=[b_barz])
        P.op("dve", lambda h: h.memset(barsc["zb"][:], 0.0), w=[b_barz])
        barsc["zbuf"] = b_barz

        with contextlib.ExitStack() as st:
            barsc["p"] = PS(st, "bar_p0", [128, 512], F32)
            b_barp0 = P.buf("barp0", excl=True)
            cT = SB(st, "cT", [128, KD], F32)
            condT = SB(st, "condT", [128, KD], F32)
            condB = SB(st, "condB", [128, KD, 128], F32)
            wa = [SB(st, f"wa{i}", [128, KD, 256], F32) for i in range(2)]
            modB = SB(st, "modB", [128, 6 * D], F32)
            baB = SB(st, "baB", [128, 6 * D], F32)
            modT = SB(st, "modT", [128, 4, KD], F32)
            ngT = SB(st, "ngT", [128, 2, KD], F32)
            pm = [PS(st, f"pm{i}", [128, 512], F32) for i in range(2)]
            ptr = [PS(st, f"ptr{i}", [128, 512], F32) for i in range(2)]
            b_cT, b_condT, b_condB, b_baB, b_modT, b_ngT = P.bufs(6, "p0")
            b_wa = P.bufs(2, "wa")
            b_pm = P.bufs(2, "pm", excl=True)
            b_ptr = P.bufs(2, "ptr", excl=True)
            b_modB = P.bufs(48, "modB")
            P.dma("sp", cT[:], c_T[:, :], w=[b_cT], key="c1")
            P.dma("sp", ngT[:, 0, :], n1g_T[:, :], w=[b_ngT], key="c2")
            P.dma("sp", ngT[:, 1, :], n2g_T[:, :], w=[b_ngT], key="c2")
            P.dma("act", baB[:], b_ada[0:1, :].broadcast_to([128, 6 * D]), w=[b_baB], key="c3")
            P.op("act", lambda h: h.activation(out=condT[:], in_=cT[:], func=AF.Silu), r=[b_cT], w=[b_condT])
            for kc in range(KD):
                P.op("dve", lambda h, kc=kc: h.tensor_copy(out=condB[:, kc, :], in_=condT[:, kc:kc + 1].to_broadcast([128, 128])),
                     r=[b_condT], w=[b_condB])
            if stop == -3:
                P.dma("sp", out[0:128, 0:128], condB[:, 3, :], r=[b_condB], key="dbg")
                P.dma("sp", out[128:256, :], baB[:, 0:D], r=[b_baB], key="dbg1")
                return nc, P.emit()
            for n in range(48 if stop != -2 else 1):
                s = n % 2
                P.dma("sp" if n % 2 == 0 else "act", wa[s][:],
                      w_ada[:, n * 256:(n + 1) * 256].rearrange("(kc p) c -> p kc c", p=128),
                      w=[b_wa[s]], key=f"wa{s}")
                for kc in range(KD):
                    P.op("pe", lambda h, kc=kc, s=s: h.matmul(pm[s][:, 0:256], lhsT=condB[:, kc, :], rhs=wa[s][:, kc, :],
                                                              start=(kc == 0), stop=(kc == KD - 1)),
                         r=[b_condB, b_wa[s]], w=[b_pm[s]])
                P.op("dve", lambda h, n=n, s=s: h.tensor_tensor(out=modB[:, n * 256:(n + 1) * 256], in0=pm[s][:, 0:256],
                                                                 in1=baB[:, n * 256:(n + 1) * 256], op=ALU.add),
                     r=[b_pm[s], b_baB], w=[b_modB[n]])
            if stop in (-2, -1):
                P.dma("sp", out[0:128, :], modB[:, 0:D], r=b_modB[0:8], key="dbg")
                return nc, P.emit()
            for vi, v in enumerate((0, 1, 3, 4)):
                for j in range(KD):
                    col = v * D + j * 128
                    s = (vi * KD + j) % 2
                    P.op("pe", lambda h, col=col, s=s: h.matmul(ptr[s][:, 0:128], lhsT=modB[:, col:col + 128], rhs=identf[:], start=True, stop=True),
                         r=[b_modB[col // 256], b_identf], w=[b_ptr[s]])
                    P.op("dve", lambda h, vi=vi, j=j, s=s: h.tensor_copy(out=modT[:, vi, j:j + 1], in_=ptr[s][:, 0:1]),
                         r=[b_ptr[s]], w=[b_modT])
            P.op("dve", lambda h: h.scalar_tensor_tensor(out=A1[:], in0=modT[:, 1, :], scalar=1.0, in1=ngT[:, 0, :], op0=ALU.add, op1=ALU.mult),
                 r=[b_modT, b_ngT], w=[b_A1])
            P.op("dve", lambda h: h.tensor_copy(out=B1[:], in_=modT[:, 0, :]), r=[b_modT], w=[b_B1])
            P.op("dve", lambda h: h.scalar_tensor_tensor(out=A2[:], in0=modT[:, 3, :], scalar=1.0, in1=ngT[:, 1, :], op0=ALU.add, op1=ALU.mult),
                 r=[b_modT, b_ngT], w=[b_A2])
            P.op("dve", lambda h: h.tensor_copy(out=B2[:], in_=modT[:, 2, :]), r=[b_modT], w=[b_B2])
            P.op("dve", lambda h: h.tensor_copy(out=gB[:], in_=modB[:, 2 * D:3 * D]), r=b_modB[16:24], w=[b_gB])
            P.dma("sp", g2_scr[:, :], modB[:, 5 * D:6 * D], r=b_modB[40:48], key="g2s")
            if stop == 0:
                P.dma("sp", out[0:128, :], gB[:], r=[b_gB], key="dbg")
                P.dma("sp", out[128:256, 0:KD], A1[:], r=[b_A1], key="dbg1")
                P.dma("sp", out[128:256, KD:2 * KD], B1[:], r=[b_B1], key="dbg2")
                return nc, P.emit()
            P.barrier(barsc, [b_barp0])

        with contextlib.ExitStack() as st:
            pg = [PS(st, f"pg{i}", [128, 512], F32) for i in range(4)]
            ptb = [PS(st, f"ptb{i}", [128, 8, 128], BF16) for i in range(2)]
            py = PS(st, "py", [128, 1024], F32)
            b_pg = P.bufs(4, "pg", excl=True)
            b_ptb = P.bufs(2, "ptb", excl=True)
            b_py = P.buf("py", excl=True)
            barsc["p"] = pg[0]
            pgi = [0]

            def next_pg():
                i = pgi[0] % 4
                pgi[0] += 1
                return pg[i], b_pg[i]
            pti = [0]

            def next_pt():
                i = pti[0] % 2
                pti[0] += 1
                return ptb[i], b_ptb[i]

            if stop == 10:
                P.dma("sp", out[0:128, :], gB[:], r=[b_gB], key="dbg")
                return nc, P.emit()
            ut = SB(st, "ut", [128, 128], F32)
            ml = SB(st, "ml", [128, 128], F32)
            ones = SB(st, "ones", [128, 128], F32)
            cw = SB(st, "cw", [128, 32, 4], F32)
            cb = SB(st, "cb", [128, 32], F32)
            dtb = SB(st, "dtb", [128, NH], F32)
            aneg = SB(st, "aneg", [128, NH], F32)
            dsk = SB(st, "dsk", [128, NH], F32)
            sgBf = None
            sgB = SB(st, "sgB", [128, D_SSD], BF16)
            wpl = SB(st, "wpl", [128, 4, 2, 256], BF16)
            bps = SB(st, "bps", [128, 8], F32)
            psc = SB(st, "psc", [128, 8], F32)
            wr = SB(st, "wr", [128, KD, NE], F32)
            brB = SB(st, "brB", [128, NE], F32)
            lastv_t = SB(st, "lastv_t", [128, 1], F32)
            invc_t = SB(st, "invc_t", [128, 4, GT], F32)
            pmask_t = SB(st, "pmask_t", [128, max(TP, 128) // 128], F32)
            b_const = P.buf("const1")
            P.dma("sp", ut[:], utd[:, :], w=[b_const], key="c4")
            P.dma("sp", ml[:], mld[:, :], w=[b_const], key="c4")
            P.dma("sp", cw[:], conv_wT[:, :, :], w=[b_const], key="c4")
            P.dma("sp", cb[:], conv_bT[:, :], w=[b_const], key="c4")
            P.dma("sp", dtb[:], dt_bias[0:1, :].broadcast_to([128, NH]), w=[b_const], key="c4")
            P.dma("sp", aneg[:], a_log[0:1, :].broadcast_to([128, NH]), w=[b_const], key="c4")
            P.dma("sp", dsk[:], d_skip[0:1, :].broadcast_to([128, NH]), w=[b_const], key="c4")
            P.dma("sp", bps[:], b_poolT[:, :], w=[b_const], key="c4")
            P.dma("sp", psc[:], pscaleT[:, :], w=[b_const], key="c4")
            P.dma("sp", wr[:], w_router.rearrange("(kc p) e -> p kc e", p=128), w=[b_const], key="c4")
            P.dma("sp", brB[:], b_router[0:1, :].broadcast_to([128, NE]), w=[b_const], key="c4")
            P.dma("sp", lastv_t[:], lastv[:, :], w=[b_const], key="c4")
            P.dma("sp", invc_t[:], invc.rearrange("(o g) t -> o g t", o=1).broadcast_to([128, 4, GT]), w=[b_const], key="c4")
            P.dma("sp", pmask_t[:], pmask[:, :], w=[b_const], key="c4")
            b_c2 = P.buf("const2")
            P.op("dve", lambda h: h.memset(ones[:], 1.0), w=[b_c2])
            P.op("act", lambda h: h.activation(out=aneg[:], in_=aneg[:], func=AF.Exp), r=[b_const], w=[b_const])
            P.op("dve", lambda h: h.tensor_scalar(out=aneg[:], in0=aneg[:], scalar1=-1.0, scalar2=None, op0=ALU.mult), r=[b_const], w=[b_const])
            P.op("dve", lambda h: h.tensor_tensor(out=bps[:], in0=bps[:], in1=psc[:], op=ALU.mult), r=[b_const], w=[b_const])

            xst = SB(st, "xst", [128, D], F32)
            xn = SB(st, "xn", [128, D], BF16)
            ss = SB(st, "ss", [128, 4], F32)
            hT = SB(st, "hT", [128, KD, GT], BF16)
            wsl = [SB(st, f"wsl{i}", [128, 4096], BF16) for i in range(2)]
            zs = SB(st, "zs", [128, GT // 128, D_SSD], BF16)
            xbcT = SB(st, "xbcT", [128, 32, GT], BF16)
            pre = [SB(st, f"pre{i}", [128, GT + 3], F32) for i in range(2)]
            cacc = [SB(st, f"cacc{i}", [128, GT], F32) for i in range(2)]
            chalo = SB(st, "chalo", [128, 32, 3], F32)
            uT = SB(st, "uT", [128, 8, GT + 15], F32)
            ptmp = [SB(st, f"ptmp{i}", [128, GT + 15], F32) for i in range(2)]
            pooledT = SB(st, "pooledT", [128, 8, GT], BF16)
            yT = SB(st, "yT", [128, 32, GT], BF16)
            dtr = SB(st, "dtr", [128, GT // 128, NH], F32)
            dA = SB(st, "dA", [128, GT // 128, NH], F32)
            cum = SB(st, "cum", [128, NH], F32)
            ecum = SB(st, "ecum", [128, NH], F32)
            wend = SB(st, "wend", [128, NH], F32)
            etot = SB(st, "etot", [128, NH], F32)
            xs = SB(st, "xs", [128, D_SSD], BF16)
            btok = SB(st, "btok", [128, 512], BF16)
            xw = SB(st, "xw", [128, D_SSD], BF16)
            lt = [SB(st, f"lt{i}", [128, 128], F32) for i in range(3)]
            dec = [SB(st, f"dec{i}", [128, 128], F32) for i in range(3)]
            cbm = SB(st, "cbm", [128, 128], F32)
            mt = [SB(st, f"mt{i}", [128, 128], BF16) for i in range(3)]
            yg = SB(st, "yg", [128, 768], F32)
            ytmp = SB(st, "ytmp", [128, 768], F32)
            yn = SB(st, "yn", [128, 768], BF16)
            state = SB(st, "state", [128, D_SSD], F32)
            stbf = SB(st, "stbf", [128, D_SSD], BF16)
            xn2 = xst
            h2f = SB(st, "h2f", [128, KD, 128], F32)
            h2b = SB(st, "h2b", [128, KD, 128], BF16)
            lg = SB(st, "lg", [128, NE], F32)
            top8 = SB(st, "top8", [128, 8], F32)
            gsm = SB(st, "gsm", [128, 4], F32)

            b_sgB = P.buf("sgB")
            b_wpl = P.buf("wpl")
            b_xst0 = P.buf("xst0")
            wpl_f = xst[:].rearrange("p (g c d) -> p g c d", g=4, c=2)
            P.dma("sp", wpl_f, w_pool.rearrange("g (c2 p) d -> p g c2 d", p=128), w=[b_xst0], key="xst0")
            P.op("dve", lambda h: h.tensor_copy(out=wpl[:], in_=wpl_f), r=[b_xst0], w=[b_wpl])
            P.dma("sp", xst[:, 0:1536].bitcast(F32) if False else xst[:, 0:D], ssd_g[0:1, 0:D].broadcast_to([128, D]), r=[b_wpl], w=[b_xst0], key="xst0")
            P.op("dve", lambda h: h.tensor_copy(out=sgB[:, 0:D], in_=xst[:, 0:D]), r=[b_xst0], w=[b_sgB])
            P.dma("sp", xst[:, 0:D_SSD - D], ssd_g[0:1, D:D_SSD].broadcast_to([128, D_SSD - D]), r=[b_sgB], w=[b_xst0], key="xst0")
            P.op("dve", lambda h: h.tensor_copy(out=sgB[:, D:D_SSD], in_=xst[:, 0:D_SSD - D]), r=[b_xst0], w=[b_sgB])
            (b_xst, b_xn, b_ss, b_hT, b_zs, b_chalo, b_uT, b_pooledT, b_dtr, b_dA, b_cum, b_ecum, b_wend, b_etot,
             b_xs_, b_btok, b_xw, b_cbm, b_yg, b_ytmp, b_yn, b_x1t, b_xn2_, b_h2f, b_h2b, b_lg, b_top8, b_gsm) = P.bufs(28, "w")
            b_xn2 = b_xst
            b_hT = P.bufs(2, "hT")
            b_xs = P.bufs(3, "xs")
            b_wsl = P.bufs(2, "wsl")
            b_xbcT = P.bufs(32, "xbcT")
            b_pre = P.bufs(2, "pre")
            b_cacc = P.bufs(2, "cacc")
            b_ptmp = P.bufs(2, "ptmp")
            b_yT = P.bufs(32, "yT")
            b_lt = P.bufs(3, "lt")
            b_dec = P.bufs(3, "dec")
            b_mt = P.bufs(3, "mt")
            b_state = P.bufs(NG, "state")
            b_stbf = P.bufs(NG, "stbf")

            if stop == 11:
                P.dma("sp", out[0:128, :], gB[:], r=[b_gB, b_const, b_c2, b_sgB, b_wpl], key="dbg")
                return nc, P.emit()
            P.op("dve", lambda h: h.memset(state[:], 0.0), w=b_state)
            P.op("dve", lambda h: h.memset(stbf[:], 0.0), w=b_stbf)
            P.op("dve", lambda h: h.memset(chalo[:], 0.0), w=[b_chalo])
            P.op("dve", lambda h: h.memset(uT[:], 0.0), w=[b_uT])
            wsi = [0]

            def load_slab(src_ap, view, dims):
                i = wsi[0] % 2
                wsi[0] += 1
                n = 1
                for d_ in dims:
                    n *= d_
                dst = wsl[i][:, 0:n]
                if len(dims) == 2:
                    dst = dst.rearrange("p (a b) -> p a b", a=dims[0])
                P.dma("pool", dst, src_ap, w=[b_wsl[i]], key=f"wsl{i}")
                return dst, b_wsl[i]

            def rms_front(src_rows, sub, Acol, Bcol, dst_hT, b_dst):
                P.dma("sp", xst[:], src_rows, r=[b_sgB], w=[b_xst], key="xst")
                P.op("act", lambda h: h.activation(out=xn[:], in_=xst[:], func=AF.Square, accum_out=ss[:, 0:1]),
                     r=[b_xst], w=[b_xn, b_ss])
                P.op("act", lambda h: h.activation(out=ss[:, 1:2], in_=ss[:, 0:1], func=AF.Sqrt, scale=1.0 / D, bias=EPS),
                     r=[b_ss], w=[b_ss])
                P.op("dve", lambda h: h.reciprocal(out=ss[:, 2:3], in_=ss[:, 1:2]), r=[b_ss], w=[b_ss])
                P.op("act", lambda h: h.activation(out=xn[:], in_=xst[:], func=AF.Copy, scale=ss[:, 2:3]),
                     r=[b_xst, b_ss], w=[b_xn])
                for half in range(2):
                    pt, bpt = next_pt()
                    for j in range(8):
                        kc = half * 8 + j
                        P.op("pe", lambda h, pt=pt, j=j, kc=kc: h.transpose(out=pt[:, j, :], in_=xn[:, kc * 128:(kc + 1) * 128], identity=identb[:]),
                             r=[b_xn, b_identb], w=[bpt])
                    for j in range(8):
                        kc = half * 8 + j
                        eng = "act" if half == 0 else "dve"
                        if eng == "act":
                            P.op("act", lambda h, pt=pt, j=j, kc=kc: h.activation(out=dst_hT[:, kc, sub * 128:(sub + 1) * 128], in_=pt[:, j, :],
                                                                                  func=AF.Identity, scale=Acol[:, kc:kc + 1], bias=Bcol[:, kc:kc + 1]),
                                 r=[bpt, b_A1, b_B1, b_A2, b_B2], w=[b_dst[0]])
                        else:
                            P.op("dve", lambda h, pt=pt, j=j, kc=kc: h.tensor_scalar(out=dst_hT[:, kc, sub * 128:(sub + 1) * 128], in0=pt[:, j, :],
                                                                                     scalar1=Acol[:, kc:kc + 1], scalar2=Bcol[:, kc:kc + 1], op0=ALU.mult, op1=ALU.add),
                                 r=[bpt, b_A1, b_B1, b_A2, b_B2], w=[b_dst[1]])

            def group(src, tok0, prefix, first_own, need_u, maskcol0):
                NS = GT // 128
                for sub in range(NS):
                    rms_front(src[tok0 + sub * 128: tok0 + (sub + 1) * 128, :], sub, A1, B1, hT, b_hT)
                if lvl == 1:
                    return
                if not prefix:
                    for s_ in range(12):
                        c0 = s_ * 256
                        sl, bsl = load_slab(w_in[:, c0:c0 + 256].rearrange("(kc p) c -> p kc c", p=128), None, (KD, 256))
                        for sub in range(NS):
                            pz, bpz = next_pg()
                            for kc in range(KD):
                                P.op("pe", lambda h, pz=pz, sl=sl, kc=kc, sub=sub: h.matmul(pz[:, 0:256], lhsT=hT[:, kc, sub * 128:(sub + 1) * 128], rhs=sl[:, kc, :],
                                                                                            start=(kc == 0), stop=(kc == KD - 1)),
                                     r=b_hT + [bsl], w=[bpz])
                            P.op("act", lambda h, pz=pz, sub=sub, c0=c0: h.activation(out=zs[:, sub, c0:c0 + 256], in_=pz[:, 0:256], func=AF.Silu),
                                 r=[bpz], w=[b_zs])
                if lvl == 2:
                    return
                for s_ in range(16):
                    if prefix and (not need_u) and s_ >= 14:
                        continue
                    c0 = OFF_XBC + s_ * 256
                    sl, bsl = load_slab(w_in[:, c0:c0 + 256].rearrange("(kc p) c -> p kc c", p=128), None, (KD, 256))
                    for q in range(2):
                        cc = s_ * 2 + q
                        px, bpx = next_pg()
                        for kc in range(KD):
                            P.op("pe", lambda h, px=px, sl=sl, kc=kc, q=q: h.matmul(px[:, 0:GT], lhsT=sl[:, kc, q * 128:(q + 1) * 128], rhs=hT[:, kc, :],
                                                                                    start=(kc == 0), stop=(kc == KD - 1)),
                                 r=b_hT + [bsl], w=[bpx])
                        i = cc % 2
                        P.op("dve", lambda h, i=i, cc=cc: h.tensor_copy(out=pre[i][:, 0:3], in_=chalo[:, cc, :]), r=[b_chalo], w=[b_pre[i]])
                        P.op("act", lambda h, i=i, px=px: h.activation(out=pre[i][:, 3:3 + GT], in_=px[:, 0:GT], func=AF.Copy), r=[bpx], w=[b_pre[i]])
                        P.op("dve", lambda h, i=i, cc=cc: h.tensor_copy(out=chalo[:, cc, :], in_=pre[i][:, GT:GT + 3]), r=[b_pre[i]], w=[b_chalo])
                        P.op("dve", lambda h, i=i, cc=cc: h.tensor_scalar(out=cacc[i][:], in0=pre[i][:, 0:GT], scalar1=cw[:, cc, 0:1], scalar2=None, op0=ALU.mult),
                             r=[b_pre[i], b_const], w=[b_cacc[i]])
                        for k in (1, 2, 3):
                            P.op("dve", lambda h, i=i, cc=cc, k=k: h.scalar_tensor_tensor(out=cacc[i][:], in0=pre[i][:, k:k + GT], scalar=cw[:, cc, k:k + 1], in1=cacc[i][:],
                                                                                          op0=ALU.mult, op1=ALU.add),
                                 r=[b_pre[i], b_const, b_cacc[i]], w=[b_cacc[i]])
                        P.op("act", lambda h, i=i, cc=cc: h.activation(out=xbcT[:, cc, :], in_=cacc[i][:], func=AF.Silu, bias=cb[:, cc:cc + 1]),
                             r=[b_cacc[i], b_const], w=[b_xbcT[cc]])
                if lvl == 3:
                    return
                sl, bsl = load_slab(w_in[:, OFF_DT:OFF_DT + NH].rearrange("(kc p) c -> p kc c", p=128), None, (KD, NH))
                for sub in range(NS):
                    pd, bpd = next_pg()
                    for kc in range(KD):
                        P.op("pe", lambda h, pd=pd, sl=sl, kc=kc, sub=sub: h.matmul(pd[:, 0:NH], lhsT=hT[:, kc, sub * 128:(sub + 1) * 128], rhs=sl[:, kc, :],
                                                                                    start=(kc == 0), stop=(kc == KD - 1)),
                             r=b_hT + [bsl], w=[bpd])
                    P.op("dve", lambda h, pd=pd, sub=sub: h.tensor_tensor(out=dtr[:, sub, :], in0=pd[:, 0:NH], in1=dtb[:], op=ALU.add), r=[bpd, b_const], w=[b_dtr])
                    P.op("act", lambda h, sub=sub: h.activation(out=dtr[:, sub, :], in_=dtr[:, sub, :], func=AF.Exp), r=[b_dtr], w=[b_dtr])
                    P.op("act", lambda h, sub=sub: h.activation(out=dtr[:, sub, :], in_=dtr[:, sub, :], func=AF.Ln, bias=1.0), r=[b_dtr], w=[b_dtr])
                    if prefix:
                        mc = maskcol0 + sub
                        P.op("dve", lambda h, sub=sub, mc=mc: h.tensor_scalar(out=dtr[:, sub, :], in0=dtr[:, sub, :], scalar1=pmask_t[:, mc:mc + 1], scalar2=None, op0=ALU.mult),
                             r=[b_dtr, b_const], w=[b_dtr])
                    P.op("dve", lambda h, sub=sub: h.tensor_tensor(out=dA[:, sub, :], in0=dtr[:, sub, :], in1=aneg[:], op=ALU.mult), r=[b_dtr, b_const], w=[b_dA])
                if lvl == 4:
                    return
                if need_u:
                    for s_ in range(4):
                        c0 = OFF_U + s_ * 256
                        sl, bsl = load_slab(w_in[:, c0:c0 + 256].rearrange("(kc p) c -> p kc c", p=128), None, (KD, 256))
                        for q in range(2):
                            cc = s_ * 2 + q
                            pu, bpu = next_pg()
                            for kc in range(KD):
                                P.op("pe", lambda h, pu=pu, sl=sl, kc=kc, q=q: h.matmul(pu[:, 0:GT], lhsT=sl[:, kc, q * 128:(q + 1) * 128], rhs=hT[:, kc, :],
                                                                                        start=(kc == 0), stop=(kc == KD - 1)),
                                     r=b_hT + [bsl], w=[bpu])
                            P.op("act", lambda h, pu=pu, cc=cc: h.activation(out=uT[:, cc, 15:15 + GT], in_=pu[:, 0:GT], func=AF.Copy), r=[bpu], w=[b_uT])
                if need_u and not prefix:
                    L = GT + 15
                    for gi in range(4):
                        wlen = 2 << gi
                        for c2 in range(2):
                            cc = 2 * gi + c2
                            cur = uT[:, cc, :]
                            bcur = b_uT
                            sh = 1
                            lo = 0
                            for step in range(gi + 1):
                                i = step % 2
                                lo2 = lo + sh
                                P.op("dve", lambda h, cur=cur, i=i, lo2=lo2, sh=sh: h.tensor_tensor(out=ptmp[i][:, lo2:L], in0=cur[:, lo2:L], in1=cur[:, lo2 - sh:L - sh], op=ALU.add),
                                     r=[bcur], w=[b_ptmp[i]])
                                cur = ptmp[i]
                                bcur = b_ptmp[i]
                                lo = lo2
                                sh *= 2
                            if first_own:
                                P.op("dve", lambda h, cur=cur, gi=gi: h.tensor_tensor(out=cur[:, 15:L], in0=cur[:, 15:L], in1=invc_t[:, gi, :], op=ALU.mult),
                                     r=[bcur, b_const], w=[bcur])
                                P.op("dve", lambda h, cur=cur, cc=cc: h.tensor_tensor(out=pooledT[:, cc, :], in0=cur[:, 15:L], in1=uT[:, cc, 15:L], op=ALU.subtract),
                                     r=[bcur, b_uT], w=[b_pooledT])
                            else:
                                P.op("dve", lambda h, cur=cur, cc=cc, wlen=wlen: h.scalar_tensor_tensor(out=pooledT[:, cc, :], in0=cur[:, 15:L], scalar=1.0 / wlen, in1=uT[:, cc, 15:L],
                                                                                                        op0=ALU.mult, op1=ALU.subtract),
                                     r=[bcur, b_uT], w=[b_pooledT])
                    for gi in range(4):
                        for dc in range(2):
                            pp, bpp = next_pg()
                            for c2 in range(2):
                                P.op("pe", lambda h, pp=pp, gi=gi, dc=dc, c2=c2: h.matmul(pp[:, 0:GT], lhsT=wpl[:, gi, c2, dc * 128:(dc + 1) * 128], rhs=pooledT[:, 2 * gi + c2, :],
                                                                                          start=(c2 == 0), stop=(c2 == 1)),
                                     r=[b_pooledT, b_wpl], w=[bpp])
                            cc = 2 * gi + dc
                            P.op("act", lambda h, pp=pp, cc=cc: h.activation(out=yT[:, 24 + cc, :], in_=pp[:, 0:GT], func=AF.Identity, scale=psc[:, cc:cc + 1], bias=bps[:, cc:cc + 1]),
                                 r=[bpp, b_const], w=[b_yT[24 + cc]])
                if need_u:
                    P.op("dve", lambda h: h.tensor_copy(out=ptmp[0][:, 0:15 * 8].rearrange("p (c t) -> p c t", c=8), in_=uT[:, :, GT:GT + 15]), r=[b_uT], w=[b_ptmp[0]])
                    P.op("dve", lambda h: h.tensor_copy(out=uT[:, :, 0:15], in_=ptmp[0][:, 0:15 * 8].rearrange("p (c t) -> p c t", c=8)), r=[b_ptmp[0]], w=[b_uT])
                if lvl == 5:
                    return
                for sub in range(NS):
                    t0 = sub * 128
                    for blk in range(3):
                        pt, bpt = next_pt()
                        for j in range(8):
                            cc = blk * 8 + j
                            P.op("pe", lambda h, pt=pt, j=j, cc=cc, t0=t0: h.transpose(out=pt[:, j, :], in_=xbcT[:, cc, t0:t0 + 128], identity=identb[:]),
                                 r=[b_xbcT[cc], b_identb], w=[bpt])
                        eng = "act" if blk % 2 == 0 else "dve"
                        if eng == "act":
                            P.op("act", lambda h, pt=pt, blk=blk: h.activation(out=xs[:, blk * 1024:(blk + 1) * 1024], in_=pt[:].rearrange("p a b -> p (a b)"), func=AF.Copy),
                                 r=[bpt], w=[b_xs[blk]])
                        else:
                            P.op("dve", lambda h, pt=pt, blk=blk: h.tensor_copy(out=xs[:, blk * 1024:(blk + 1) * 1024], in_=pt[:].rearrange("p a b -> p (a b)")),
                                 r=[bpt], w=[b_xs[blk]])
                    pt, bpt = next_pt()
                    for j in range(4):
                        cc = 24 + j
                        P.op("pe", lambda h, pt=pt, j=j, cc=cc, t0=t0: h.transpose(out=pt[:, j, :], in_=xbcT[:, cc, t0:t0 + 128], identity=identb[:]),
                             r=[b_xbcT[cc], b_identb], w=[bpt])
                    P.op("dve", lambda h, pt=pt: h.tensor_copy(out=btok[:], in_=pt[:, 0:4, :].rearrange("p a b -> p (a b)")), r=[bpt], w=[b_btok])
                    pc, bpc = next_pg()
                    P.op("pe", lambda h, pc=pc, sub=sub: h.matmul(pc[:, 0:NH], lhsT=ut[:], rhs=dA[:, sub, :], start=True, stop=True), r=[b_dA, b_const], w=[bpc])
                    P.op("pe", lambda h, pc=pc, sub=sub: h.matmul(pc[:, 64:64 + NH], lhsT=ones[:], rhs=dA[:, sub, :], start=True, stop=True), r=[b_dA, b_c2], w=[bpc])
                    P.op("act", lambda h, pc=pc: h.activation(out=cum[:], in_=pc[:, 0:NH], func=AF.Copy), r=[bpc], w=[b_cum])
                    P.op("act", lambda h, pc=pc: h.activation(out=etot[:], in_=pc[:, 64:64 + NH], func=AF.Exp), r=[bpc], w=[b_etot])
                    P.op("dve", lambda h, pc=pc: h.tensor_tensor(out=wend[:], in0=pc[:, 64:64 + NH], in1=cum[:], op=ALU.subtract), r=[bpc, b_cum], w=[b_wend])
                    P.op("act", lambda h: h.activation(out=wend[:], in_=wend[:], func=AF.Exp), r=[b_wend], w=[b_wend])
                    P.op("dve", lambda h, sub=sub: h.tensor_tensor(out=wend[:], in0=wend[:], in1=dtr[:, sub, :], op=ALU.mult), r=[b_wend, b_dtr], w=[b_wend])
                    if not prefix:
                        P.op("act", lambda h: h.activation(out=ecum[:], in_=cum[:], func=AF.Exp), r=[b_cum], w=[b_ecum])
                    for g in range(NG):
                        gs = slice(g * 768, (g + 1) * 768)
                        hs = slice(g * HPG, (g + 1) * HPG)
                        if not prefix:
                            pcb, bpcb = next_pg()
                            P.op("pe", lambda h, pcb=pcb, g=g, t0=t0: h.matmul(pcb[:, 0:128], lhsT=xbcT[:, 24 + g, t0:t0 + 128], rhs=xbcT[:, 28 + g, t0:t0 + 128], start=True, stop=True),
                                 r=[b_xbcT[24 + g], b_xbcT[28 + g]], w=[bpcb])
                            P.op("dve", lambda h, pcb=pcb: h.tensor_tensor(out=cbm[:], in0=pcb[:, 0:128], in1=ut[:], op=ALU.mult), r=[bpcb, b_const], w=[b_cbm])
                            for r_ in range(HPG):
                                hh = g * HPG + r_
                                i = hh % 3
                                P.op("dve", lambda h, i=i, hh=hh, sub=sub: h.tensor_scalar(out=lt[i][:], in0=ml[:], scalar1=dA[:, sub, hh:hh + 1], scalar2=None, op0=ALU.mult),
                                     r=[b_dA, b_const], w=[b_lt[i]])
                                psg, bpsg = next_pg()
                                P.op("pe", lambda h, psg=psg, i=i: h.matmul(psg[:, 0:128], lhsT=lt[i][:], rhs=ut[:], start=True, stop=True), r=[b_lt[i], b_const], w=[bpsg])
                                P.op("act", lambda h, psg=psg, i=i: h.activation(out=dec[i][:], in_=psg[:, 0:128], func=AF.Exp), r=[bpsg], w=[b_dec[i]])
                                P.op("dve", lambda h, i=i, hh=hh, sub=sub: h.scalar_tensor_tensor(out=mt[i][:], in0=dec[i][:], scalar=dtr[:, sub, hh:hh + 1], in1=cbm[:], op0=ALU.mult, op1=ALU.mult),
                                     r=[b_dec[i], b_dtr, b_cbm], w=[b_mt[i]])
                                P.op("pe", lambda h, i=i, hh=hh, r_=r_: h.matmul(py[:, r_ * 64:(r_ + 1) * 64], lhsT=mt[i][:], rhs=xs[:, hh * 64:(hh + 1) * 64], start=True, stop=True),
                                     r=[b_mt[i]] + b_xs, w=[b_py])
                            po1, bpo1 = next_pg()
                            po2, bpo2 = next_pg()
                            P.op("pe", lambda h, po1=po1, g=g, t0=t0: h.matmul(po1[:, 0:512], lhsT=xbcT[:, 28 + g, t0:t0 + 128], rhs=stbf[:, g * 768:g * 768 + 512], start=True, stop=True),
                                 r=[b_xbcT[28 + g], b_stbf[g]], w=[bpo1])
                            P.op("pe", lambda h, po2=po2, g=g, t0=t0: h.matmul(po2[:, 0:256], lhsT=xbcT[:, 28 + g, t0:t0 + 128], rhs=stbf[:, g * 768 + 512:(g + 1) * 768], start=True, stop=True),
                                 r=[b_xbcT[28 + g], b_stbf[g]], w=[bpo2])
                            P.op("dve", lambda h, po1=po1, g=g: h.tensor_tensor(out=ytmp[:, 0:512].rearrange("p (a b) -> p a b", b=64), in0=po1[:, 0:512].rearrange("p (a b) -> p a b", b=64),
                                                                                in1=ecum[:, g * HPG:g * HPG + 8].unsqueeze(2).broadcast_to([128, 8, 64]), op=ALU.mult),
                                 r=[bpo1, b_ecum], w=[b_ytmp])
                            P.op("dve", lambda h, po2=po2, g=g: h.tensor_tensor(out=ytmp[:, 512:768].rearrange("p (a b) -> p a b", b=64), in0=po2[:, 0:256].rearrange("p (a b) -> p a b", b=64),
                                                                                in1=ecum[:, g * HPG + 8:(g + 1) * HPG].unsqueeze(2).broadcast_to([128, 4, 64]), op=ALU.mult),
                                 r=[bpo2, b_ecum], w=[b_ytmp])
                            P.op("dve", lambda h: h.tensor_tensor(out=yg[:], in0=py[:, 0:768], in1=ytmp[:], op=ALU.add), r=[b_py, b_ytmp], w=[b_yg])
                            P.op("dve", lambda h, gs=gs, hs=hs: h.tensor_tensor(out=ytmp[:].rearrange("p (a b) -> p a b", b=64), in0=xs[:, gs].rearrange("p (a b) -> p a b", b=64),
                                                                                 in1=dsk[:, hs].unsqueeze(2).broadcast_to([128, HPG, 64]), op=ALU.mult),
                                 r=b_xs + [b_const], w=[b_ytmp])
                            P.op("dve", lambda h: h.tensor_tensor(out=yg[:], in0=yg[:], in1=ytmp[:], op=ALU.add), r=[b_yg, b_ytmp], w=[b_yg])
                            P.op("dve", lambda h, sub=sub, gs=gs: h.tensor_tensor(out=yg[:], in0=yg[:], in1=zs[:, sub, gs], op=ALU.mult), r=[b_yg, b_zs], w=[b_yg])
                            P.op("act", lambda h: h.activation(out=ytmp[:], in_=yg[:], func=AF.Square, accum_out=gsm[:, 0:1]), r=[b_yg], w=[b_ytmp, b_gsm])
                            P.op("act", lambda h: h.activation(out=gsm[:, 1:2], in_=gsm[:, 0:1], func=AF.Sqrt, scale=1.0 / 768, bias=EPS), r=[b_gsm], w=[b_gsm])
                            P.op("dve", lambda h: h.reciprocal(out=gsm[:, 2:3], in_=gsm[:, 1:2]), r=[b_gsm], w=[b_gsm])
                            P.op("dve", lambda h, gs=gs: h.scalar_tensor_tensor(out=yn[:], in0=yg[:], scalar=gsm[:, 2:3], in1=sgB[:, gs], op0=ALU.mult, op1=ALU.mult),
                                 r=[b_yg, b_gsm, b_sgB], w=[b_yn])
                            pt, bpt = next_pt()
                            for j in range(6):
                                P.op("pe", lambda h, pt=pt, j=j: h.transpose(out=pt[:, j, :], in_=yn[:, j * 128:(j + 1) * 128], identity=identb[:]), r=[b_yn, b_identb], w=[bpt])
                            for j in range(6):
                                cc = 6 * g + j
                                P.op("act" if g % 2 == 0 else "dve",
                                     (lambda h, pt=pt, j=j, cc=cc, t0=t0: h.activation(out=yT[:, cc, t0:t0 + 128], in_=pt[:, j, :], func=AF.Copy)) if g % 2 == 0 else
                                     (lambda h, pt=pt, j=j, cc=cc, t0=t0: h.tensor_copy(out=yT[:, cc, t0:t0 + 128], in_=pt[:, j, :])),
                                     r=[bpt], w=[b_yT[cc]])
                        P.op("dve", lambda h, gs=gs, hs=hs: h.tensor_tensor(out=xw[:, gs].rearrange("p (a b) -> p a b", b=64), in0=xs[:, gs].rearrange("p (a b) -> p a b", b=64),
                                                                             in1=wend[:, hs].unsqueeze(2).broadcast_to([128, HPG, 64]), op=ALU.mult),
                             r=b_xs + [b_wend], w=[b_xw])
                        pq1, bpq1 = next_pg()
                        pq2, bpq2 = next_pg()
                        P.op("pe", lambda h, pq1=pq1, g=g: h.matmul(pq1[:, 0:512], lhsT=btok[:, g * 128:(g + 1) * 128], rhs=xw[:, g * 768:g * 768 + 512], start=True, stop=True),
                             r=[b_btok, b_xw], w=[bpq1])
                        P.op("pe", lambda h, pq2=pq2, g=g: h.matmul(pq2[:, 0:256], lhsT=btok[:, g * 128:(g + 1) * 128], rhs=xw[:, g * 768 + 512:(g + 1) * 768], start=True, stop=True),
                             r=[b_btok, b_xw], w=[bpq2])
                        P.op("dve", lambda h, gs=gs, hs=hs: h.tensor_tensor(out=state[:, gs].rearrange("p (a b) -> p a b", b=64), in0=state[:, gs].rearrange("p (a b) -> p a b", b=64),
                                                                            in1=etot[:, hs].unsqueeze(2).broadcast_to([128, HPG, 64]), op=ALU.mult),
                             r=[b_state[g], b_etot], w=[b_state[g]])
                        P.op("dve", lambda h, pq1=pq1, g=g: h.tensor_tensor(out=state[:, g * 768:g * 768 + 512], in0=state[:, g * 768:g * 768 + 512], in1=pq1[:, 0:512], op=ALU.add),
                             r=[b_state[g], bpq1], w=[b_state[g]])
                        P.op("dve", lambda h, pq2=pq2, g=g: h.tensor_tensor(out=state[:, g * 768 + 512:(g + 1) * 768], in0=state[:, g * 768 + 512:(g + 1) * 768], in1=pq2[:, 0:256], op=ALU.add),
                             r=[b_state[g], bpq2], w=[b_state[g]])
                        P.op("act", lambda h, gs=gs: h.activation(out=stbf[:, gs], in_=state[:, gs], func=AF.Copy), r=[b_state[g]], w=[b_stbf[g]])
                if prefix:
                    return
                if lvl == 6:
                    return
                x1acc = {}
                for sub in range(NS):
                    pass
                for os_ in range(16):
                    c0 = os_ * 128
                    sl, bsl = load_slab(w_out[:, c0:c0 + 128].rearrange("(cc p) c -> p cc c", p=128), None, (32, 128))
                    for sub in range(NS):
                        po, bpo = next_pg()
                        for cc in range(32):
                            P.op("pe", lambda h, po=po, sl=sl, cc=cc, sub=sub: h.matmul(po[:, 0:128], lhsT=yT[:, cc, sub * 128:(sub + 1) * 128], rhs=sl[:, cc, :],
                                                                                        start=(cc == 0), stop=(cc == 31)),
                                 r=[b_yT[cc], bsl], w=[bpo])
                        P.op("dve", lambda h, po=po, c0=c0, sub=sub: h.tensor_tensor(out=x1g[sub][:, c0:c0 + 128], in0=po[:, 0:128], in1=gB[:, c0:c0 + 128], op=ALU.mult),
                             r=[bpo, b_gB], w=[b_x1g[sub]])
                for sub in range(NS):
                    tok = tok0 + sub * 128
                    tile_i = tok // 128
                    P.dma("sp", xst[:], src[tok:tok + 128, :], w=[b_xst], key="xst")
                    P.op("dve", lambda h, sub=sub: h.tensor_tensor(out=x1g[sub][:], in0=x1g[sub][:], in1=xst[:], op=ALU.add), r=[b_x1g[sub], b_xst], w=[b_x1g[sub]])
                    P.dma("sp", x1_scr[tok:tok + 128, :], x1g[sub][:], r=[b_x1g[sub]], key=f"x1s{sub}")
                    if lvl == 7:
                        continue
                    P.op("act", lambda h, sub=sub: h.activation(out=xn2[:], in_=x1g[sub][:], func=AF.Square, accum_out=ss[:, 0:1]), r=[b_x1g[sub]], w=[b_xn2, b_ss])
                    P.op("act", lambda h: h.activation(out=ss[:, 1:2], in_=ss[:, 0:1], func=AF.Sqrt, scale=1.0 / D, bias=EPS), r=[b_ss], w=[b_ss])
                    P.op("dve", lambda h: h.reciprocal(out=ss[:, 2:3], in_=ss[:, 1:2]), r=[b_ss], w=[b_ss])
                    P.op("act", lambda h, sub=sub: h.activation(out=xn2[:], in_=x1g[sub][:], func=AF.Copy, scale=ss[:, 2:3]), r=[b_x1g[sub], b_ss], w=[b_xn2])
                    for q in range(4):
                        pf, bpf = next_pg()
                        for j in range(4):
                            kc = q * 4 + j
                            P.op("pe", lambda h, pf=pf, j=j, kc=kc: h.matmul(pf[:, j * 128:(j + 1) * 128], lhsT=xn2[:, kc * 128:(kc + 1) * 128], rhs=identf[:], start=True, stop=True),
                                 r=[b_xn2, b_identf], w=[bpf])
                        for j in range(4):
                            kc = q * 4 + j
                            P.op("act", lambda h, pf=pf, j=j, kc=kc: h.activation(out=h2f[:, kc, :], in_=pf[:, j * 128:(j + 1) * 128], func=AF.Identity,
                                                                                  scale=A2[:, kc:kc + 1], bias=B2[:, kc:kc + 1]),
                                 r=[bpf, b_A2, b_B2], w=[b_h2f])
                    P.op("dve", lambda h: h.tensor_copy(out=h2b[:], in_=h2f[:]), r=[b_h2f], w=[b_h2b])
                    P.dma("sp", h2T_scr[:, :, tok:tok + 128], h2b[:], r=[b_h2b], key="h2s")
                    if lvl == 8:
                        continue
                    pl, bpl = next_pg()
                    for kc in range(KD):
                        P.op("pe", lambda h, pl=pl, kc=kc: h.matmul(pl[:, 0:NE], lhsT=h2f[:, kc, :], rhs=wr[:, kc, :], start=(kc == 0), stop=(kc == KD - 1)),
                             r=[b_h2f, b_const], w=[bpl])
                    P.op("dve", lambda h, pl=pl: h.tensor_tensor(out=lg[:], in0=pl[:, 0:NE], in1=brB[:], op=ALU.add), r=[bpl, b_const], w=[b_lg])
                    P.op("dve", lambda h: h.max(out=top8[:], in_=lg[:]), r=[b_lg], w=[b_top8])
                    P.op("dve", lambda h: h.tensor_scalar(out=gsm[:, 3:4], in0=top8[:, 0:1], scalar1=-1.0, scalar2=None, op0=ALU.mult), r=[b_top8], w=[b_gsm])
                    gt_ = gates[:, tile_i, :]
                    bg_ = b_gates[tile_i]
                    P.op("act", lambda h, gt_=gt_: h.activation(out=gt_, in_=lg[:], func=AF.Exp, bias=gsm[:, 3:4]), r=[b_lg, b_gsm], w=[bg_])
                    P.op("dve", lambda h: h.tensor_scalar(out=lg[:], in0=lg[:], scalar1=top8[:, 3:4], scalar2=None, op0=ALU.is_ge), r=[b_lg, b_top8, bg_], w=[b_lg])
                    P.op("dve", lambda h, gt_=gt_: h.tensor_tensor(out=gt_, in0=gt_, in1=lg[:], op=ALU.mult), r=[bg_, b_lg], w=[bg_])
                    P.op("dve", lambda h, gt_=gt_: h.reduce_sum(out=gsm[:, 0:1], in_=gt_, axis=mybir.AxisListType.X), r=[bg_], w=[b_gsm])
                    P.op("dve", lambda h: h.reciprocal(out=gsm[:, 1:2], in_=gsm[:, 0:1]), r=[b_gsm], w=[b_gsm])
                    P.op("dve", lambda h, gt_=gt_: h.tensor_scalar(out=gt_, in0=gt_, scalar1=gsm[:, 1:2], scalar2=None, op0=ALU.mult), r=[bg_, b_gsm], w=[bg_])

            x1g = [SB(st, f"x1g{i}", [128, D], F32) for i in range(GT // 128)]
            b_x1g = P.bufs(GT // 128, "x1g")

            ngp = TP // GT
            for gi_ in range(ngp):
                last = (gi_ == ngp - 1)
                group(x_prev, gi_ * GT, True, False, last, gi_ * (GT // 128))
            if ngp > 0:
                P.op("dve", lambda h: h.tensor_scalar(out=chalo[:], in0=chalo[:], scalar1=lastv_t[:, 0:1], scalar2=None, op0=ALU.mult), r=[b_chalo, b_const], w=[b_chalo])
                P.op("dve", lambda h: h.tensor_scalar(out=uT[:, :, 0:15], in0=uT[:, :, 0:15], scalar1=lastv_t[:, 0:1], scalar2=None, op0=ALU.mult), r=[b_uT, b_const], w=[b_uT])
            for gi_ in range(T // GT):
                group(x_own, gi_ * GT, False, gi_ == 0, True, 0)
            if stop == 1:
                b_dd = P.buf("dd")
                for tt in range(T // 128):
                    P.dma("sp", xst[:], x1_scr[tt * 128:(tt + 1) * 128, :], w=[b_dd], r=[b_xst], key="dbg3")
                    P.dma("sp", out[tt * 128:(tt + 1) * 128, :], xst[:], r=[b_dd], key="dbg4")
                return nc, P.emit()
            P.barrier(barsc, [b_pg[0]])

        with contextlib.ExitStack() as st:
            NTP = PT // 128
            NHALF = PT // 512
            pgl = [PS(st, f"pgl{i}", [128, 512], F32) for i in range(4)]
            pov = [PS(st, f"pov{i}", [128, 512], F32) for i in range(4)]
            b_pgl = P.bufs(4, "pgl", excl=True)
            b_pov = P.bufs(4, "pov", excl=True)
            barsc["p"] = pgl[0]
            h2T = SB(st, "h2T", [128, KD, PT], BF16)
            acc = SB(st, "acc", [128, NTP, D], F32)
            actraw = SB(st, "actraw", [128, 16 * PT], BF16)
            actT = actraw[:].rearrange("p (f t) -> p f t", f=16)
            fngB = actraw[:, 0:2 * D].bitcast(F32)
            x1l = actraw[:, 2 * D:4 * D].bitcast(F32)
            wi = [SB(st, f"wi{i}", [128, KD, 256], BF16) for i in range(2)]
            wo = [SB(st, f"wo{i}", [128, 16, 512], BF16) for i in range(2)]
            beT = SB(st, "beT", [128, NE, 16, 2], F32)
            beL = SB(st, "beL", [128, NE, 16], F32)
            bo = actraw[0:NE, 6 * D:8 * D].bitcast(F32)
            gT = SB(st, "gT", [NE, 128], F32)
            tg = [SB(st, f"tg{i}", [128, 512], BF16) for i in range(2)]
            tsg = [SB(st, f"tsg{i}", [128, 512], BF16) for i in range(2)]
            tl = [SB(st, f"tl{i}", [128, 512], BF16) for i in range(2)]
            tt_ = [SB(st, f"tt{i}", [128, 512], BF16) for i in range(2)]
            ss2 = SB(st, "ss2", [128, 4], F32)
            b_h2T, b_beT, b_bo, b_gT, b_fngB, b_x1l, b_ss2, b_g2 = P.bufs(8, "m")
            b_acc = P.bufs(NTP, "acc")
            b_actT = P.bufs(16 * NHALF, "actT")
            b_wi = P.bufs(2, "wi")
            b_wo = P.bufs(2, "wo")
            b_tg = P.bufs(2, "tg")
            b_tsg = P.bufs(2, "tsg")
            b_tl = P.bufs(2, "tl")
            b_tt = P.bufs(2, "tt")
            P.dma("sp", beT[:], b_einT[:, :, :, :], w=[b_beT], key="m0")
            P.op("dve", lambda h: h.tensor_scalar(out=beL[:], in0=beT[:, :, :, 1], scalar1=1.0, scalar2=None, op0=ALU.add), r=[b_beT], w=[b_beT])
            P.dma("sp", gB[:], g2_scr[:, :], w=[b_g2], key="m3")
            cnt = dict(gl=0, ov=0, wi=0, wo=0, ep=0)
            for ps_ in range(T // PT):
                tokp = ps_ * PT
                P.dma("sp", h2T[:], h2T_scr[:, :, tokp:tokp + PT], w=[b_h2T], key="h2l")
                P.dma("sp", bo, b_eout[:, :], r=[b_x1l, b_fngB], w=[b_bo] + b_actT, key="m1")
                for tt in range(NTP):
                    tile_i = tokp // 128 + tt
                    pv = pov[cnt["ov"] % 4]; bpv = b_pov[cnt["ov"] % 4]; cnt["ov"] += 1
                    P.op("pe", lambda h, pv=pv, tile_i=tile_i: h.matmul(pv[0:NE, 0:128], lhsT=gates[:, tile_i, :], rhs=identf[:], start=True, stop=True),
                         r=[b_gates[tile_i], b_identf], w=[bpv])
                    P.op("act", lambda h, pv=pv: h.activation(out=gT[:], in_=pv[0:NE, 0:128], func=AF.Copy), r=[bpv], w=[b_gT])
                    for q in range(4):
                        pv2 = pov[cnt["ov"] % 4]; bpv2 = b_pov[cnt["ov"] % 4]; cnt["ov"] += 1
                        P.op("pe", lambda h, pv2=pv2, q=q: h.matmul(pv2[:, 0:512], lhsT=gT[:], rhs=bo[:, q * 512:(q + 1) * 512], start=True, stop=True),
                             r=[b_gT, b_bo], w=[bpv2])
                        P.op("act", lambda h, pv2=pv2, tt=tt, q=q: h.activation(out=acc[:, tt, q * 512:(q + 1) * 512], in_=pv2[:, 0:512], func=AF.Copy),
                             r=[bpv2], w=[b_acc[tt]])
                for e in range(NE):
                    for fc in range(16):
                        i = cnt["wi"] % 2; cnt["wi"] += 1
                        P.dma("pool", wi[i][:], w_ein[e, :, fc * 256:(fc + 1) * 256].rearrange("(kc p) c -> p kc c", p=128), w=[b_wi[i]], key=f"wi{i}")
                        wv = wi[i][:].rearrange("p k (f two) -> p k f two", two=2)
                        pgs = []
                        for hf in range(NHALF):
                            pgs.append((pgl[cnt["gl"] % 4], b_pgl[cnt["gl"] % 4])); cnt["gl"] += 1
                        for kc in range(KD):
                            for hf in range(NHALF):
                                pgt, bpg_ = pgs[hf]
                                P.op("pe", lambda h, pgt=pgt, wv=wv, kc=kc, hf=hf: h.matmul(pgt[:], lhsT=wv[:, kc, :, 0], rhs=h2T[:, kc, hf * 512:(hf + 1) * 512],
                                                                                           start=(kc == 0), stop=(kc == KD - 1)),
                                     r=[b_wi[i], b_h2T], w=[bpg_])
                        ks = []
                        for hf in range(NHALF):
                            pgt, bpg_ = pgs[hf]
                            k = cnt["ep"] % 2; cnt["ep"] += 1
                            ks.append(k)
                            P.op("dve", lambda h, pgt=pgt, k=k, e=e, fc=fc: h.tensor_scalar(out=tg[k][:], in0=pgt[:], scalar1=beT[:, e, fc, 0:1], scalar2=LIMIT, op0=ALU.add, op1=ALU.min),
                                 r=[bpg_, b_beT], w=[b_tg[k]])
                            P.op("act", lambda h, k=k: h.activation(out=tsg[k][:], in_=tg[k][:], func=AF.Sigmoid, scale=ALPHA), r=[b_tg[k]], w=[b_tsg[k]])
                            P.op("dve", lambda h, k=k: h.tensor_tensor(out=tt_[k][:], in0=tg[k][:], in1=tsg[k][:], op=ALU.mult), r=[b_tg[k], b_tsg[k]], w=[b_tt[k]])
                        pls = []
                        for hf in range(NHALF):
                            pls.append((pgl[cnt["gl"] % 4], b_pgl[cnt["gl"] % 4])); cnt["gl"] += 1
                        for kc in range(KD):
                            for hf in range(NHALF):
                                plt, bpl_ = pls[hf]
                                P.op("pe", lambda h, plt=plt, wv=wv, kc=kc, hf=hf: h.matmul(plt[:], lhsT=wv[:, kc, :, 1], rhs=h2T[:, kc, hf * 512:(hf + 1) * 512],
                                                                                           start=(kc == 0), stop=(kc == KD - 1)),
                                     r=[b_wi[i], b_h2T], w=[bpl_])
                        for hf in range(NHALF):
                            plt, bpl_ = pls[hf]
                            k = ks[hf]
                            P.op("dve", lambda h, plt=plt, k=k, e=e, fc=fc: h.tensor_scalar(out=tl[k][:], in0=plt[:], scalar1=beL[:, e, fc:fc + 1], scalar2=LIMIT + 1.0, op0=ALU.add, op1=ALU.min),
                                 r=[bpl_, b_beT], w=[b_tl[k]])
                            P.op("dve", lambda h, k=k, fc=fc, hf=hf: h.scalar_tensor_tensor(out=actT[:, fc, hf * 512:(hf + 1) * 512], in0=tl[k][:], scalar=1.0 - LIMIT, in1=tt_[k][:],
                                                                                            op0=ALU.max, op1=ALU.mult),
                                 r=[b_tt[k], b_tl[k]], w=[b_actT[fc * NHALF + hf], b_x1l, b_fngB, b_bo])
                    for os_ in range(4):
                        i = cnt["wo"] % 2; cnt["wo"] += 1
                        P.dma("pool", wo[i][:], w_eout[e, :, os_ * 512:(os_ + 1) * 512].rearrange("(fc p) c -> p fc c", p=128), w=[b_wo[i]], key=f"wo{i}")
                        for tt in range(NTP):
                            tile_i = tokp // 128 + tt
                            pv = pov[cnt["ov"] % 4]; bpv = b_pov[cnt["ov"] % 4]; cnt["ov"] += 1
                            hf = (tt * 128) // 512
                            for fc in range(16):
                                P.op("pe", lambda h, pv=pv, i=i, fc=fc, tt=tt: h.matmul(pv[:, 0:512], lhsT=actT[:, fc, tt * 128:(tt + 1) * 128], rhs=wo[i][:, fc, :],
                                                                                        start=(fc == 0), stop=(fc == 15)),
                                     r=[b_actT[fc * NHALF + hf], b_wo[i]], w=[bpv])
                            P.op("dve", lambda h, pv=pv, tt=tt, os_=os_, tile_i=tile_i, e=e: h.scalar_tensor_tensor(
                                out=acc[:, tt, os_ * 512:(os_ + 1) * 512], in0=pv[:, 0:512], scalar=gates[:, tile_i, e:e + 1],
                                in1=acc[:, tt, os_ * 512:(os_ + 1) * 512], op0=ALU.mult, op1=ALU.add),
                                 r=[bpv, b_gates[tile_i], b_acc[tt]], w=[b_acc[tt]])
                P.dma("sp", fngB, fng[0:1, :].broadcast_to([128, D]), r=b_actT, w=[b_fngB] + b_actT, key="m2")
                for tt in range(NTP):
                    tok = tokp + tt * 128
                    P.dma("sp", x1l, x1_scr[tok:tok + 128, :], r=b_actT, w=[b_x1l], key="x1l")
                    P.op("dve", lambda h, tt=tt: h.tensor_tensor(out=acc[:, tt, :], in0=acc[:, tt, :], in1=gB[:], op=ALU.mult), r=[b_acc[tt], b_g2], w=[b_acc[tt]])
                    P.op("dve", lambda h, tt=tt: h.tensor_tensor(out=x1l, in0=x1l, in1=acc[:, tt, :], op=ALU.add), r=[b_acc[tt], b_x1l], w=[b_x1l])
                    P.op("act", lambda h, tt=tt: h.activation(out=acc[:, tt, :], in_=x1l, func=AF.Square, accum_out=ss2[:, 0:1]), r=[b_x1l], w=[b_acc[tt], b_ss2])
                    P.op("act", lambda h: h.activation(out=ss2[:, 1:2], in_=ss2[:, 0:1], func=AF.Sqrt, scale=1.0 / D, bias=EPS), r=[b_ss2], w=[b_ss2])
                    P.op("dve", lambda h: h.reciprocal(out=ss2[:, 2:3], in_=ss2[:, 1:2]), r=[b_ss2], w=[b_ss2])
                    P.op("dve", lambda h, tt=tt: h.scalar_tensor_tensor(out=acc[:, tt, :], in0=x1l, scalar=ss2[:, 2:3], in1=fngB, op0=ALU.mult, op1=ALU.mult),
                         r=[b_x1l, b_ss2, b_fngB], w=[b_acc[tt]])
                    P.dma("sp", out[tok:tok + 128, :], acc[:, tt, :], r=[b_acc[tt]], key=f"outst{tt}")
            stats = P.emit()
    return nc, stats


def prep_inputs(cfg, inp):
    T, TP, NC, NE = cfg.T, cfg.TP, cfg.NC, cfg.NE
    f = np.float32
    x = np.ascontiguousarray(np.asarray(inp["x"], f)[0])
    colT = lambda v: np.ascontiguousarray(np.asarray(v, f).reshape(-1, 128).T)
    shared = dict(
        c_T=colT(inp["c"][0]),
        w_ada=np.asarray(inp["w_ada"], f)[0],
        b_ada=np.asarray(inp["b_ada"], f)[0][None, :],
        n1g_T=colT(inp["norm1_g"][0]),
        n2g_T=colT(inp["norm2_g"][0]),
        w_in=np.asarray(inp["w_in_proj"], f)[0],
        conv_wT=np.ascontiguousarray(np.asarray(inp["conv_w"], f)[0].T.reshape(32, 128, 4).transpose(1, 0, 2)),
        conv_bT=colT(inp["conv_b"][0]),
        dt_bias=np.asarray(inp["dt_bias"], f)[0][None, :],
        a_log=np.asarray(inp["a_log"], f)[0][None, :],
        d_skip=np.asarray(inp["d_skip"], f)[0][None, :],
        ssd_g=np.asarray(inp["ssd_norm_g"], f)[0][None, :],
        w_pool=np.asarray(inp["w_pool"], f)[0],
        b_poolT=colT(inp["b_pool"][0]),
        pscaleT=colT(inp["pool_scale"][0]),
        w_out=np.asarray(inp["w_out_proj"], f)[0],
        w_router=np.asarray(inp["w_router"], f)[0],
        b_router=np.asarray(inp["b_router"], f)[0][None, :],
        w_ein=np.asarray(inp["w_exp_in"], f)[0],
        b_einT=np.ascontiguousarray(np.asarray(inp["b_exp_in"], f)[0].reshape(NE, 16, 128, 2).transpose(2, 0, 1, 3)),
        w_eout=np.asarray(inp["w_exp_out"], f)[0],
        b_eout=np.asarray(inp["b_exp_out"], f)[0],
        fng=np.asarray(inp["final_norm_g"], f)[None, :],
        ident=np.eye(128, dtype=f),
        ut_c=np.triu(np.ones((128, 128), f)),
        ml_c=np.tril(np.ones((128, 128), f), -1),
    )
    maps = []
    TPp = max(TP, 128)
    for c in range(NC):
        m = dict(shared)
        m["x_own"] = x[c * T:(c + 1) * T]
        xp = np.zeros((TPp, D), f)
        pm = np.zeros((TPp,), f)
        nprev = c * T
        if nprev > 0:
            xp[TP - nprev:TP] = x[0:nprev]
            pm[TP - nprev:TP] = 1.0
        m["x_prev"] = xp
        m["pmask"] = np.ascontiguousarray(pm.reshape(-1, 128).T)
        m["lastv"] = np.full((128, 1), 1.0 if c > 0 else 0.0, f)
        ic = np.zeros((4, GT), f)
        for gi in range(4):
            w = 2 << gi
            tpos = np.arange(GT) + c * T + 1
            ic[gi] = 1.0 / np.minimum(tpos, w)
        m["invc"] = ic
        maps.append(m)
    return maps


_CACHE = {}


def run(cfg, inp):
    key = (cfg.SEQ, cfg.NC, cfg.NE, cfg.PT)
    if key not in _CACHE:
        _CACHE[key] = build_program(cfg)
    nc, stats = _CACHE[key]
    maps = prep_inputs(cfg, inp)
    res = run_bass_kernel_spmd(nc, maps, core_ids=list(range(cfg.NC)))
    outs = [np.asarray(r["out"]) for r in res.results]
    return np.concatenate(outs, axis=0)[None].astype(np.float32)


NCORES_FULL = 4
PT_FULL = 1024


def kernel(**inputs):
    cfg = Cfg(16384, NCORES_FULL, 32, PT_FULL)
    return run(cfg, inputs)
```
